# Optimizing a Trainium2 kernel written in Bass

```python
import math
import jax, jax.numpy as jnp
from jax import lax
import numpy as np

D_MODEL = 1024
BATCH = 2
SEQ = 8192
DEPTH = 1

HEAD_DIM = 64
DSA_HEADS = 8
DIFF_HEADS = 4
DIFF_DIM = 64
IDX_HEADS = 8
IDX_DIM = 64
TOPK_MAX = 256
D_FF = 4 * D_MODEL
ROPE_THETA = 10000.0
EPS = 1e-6
Q_BLOCK = 128
NEG = -1e30

DSA_W = DSA_HEADS * HEAD_DIM
DIFF_W = DIFF_HEADS * 2 * DIFF_DIM
MIX_W = DSA_W + DIFF_W
IN_SIZES = (DSA_W, DSA_W, DSA_W,
            IDX_HEADS * IDX_DIM, IDX_DIM, IDX_HEADS,
            DIFF_W, DIFF_W, DIFF_W)
IN_COLS = int(sum(IN_SIZES))
SPLIT_POINTS = tuple(int(v) for v in np.cumsum(IN_SIZES)[:-1])

kernel_name = "hybrid_dsa_diffattn_sandwich_block"

f32 = jnp.float32


def rmsnorm(x, g):
    xf = x.astype(f32)
    y = xf * lax.rsqrt(jnp.mean(xf * xf, axis=-1, keepdims=True) + EPS)
    return (y * g.astype(f32)).astype(x.dtype)


def rope_tables(seq, dim):
    inv = 1.0 / (ROPE_THETA ** (jnp.arange(0, dim, 2, dtype=f32) / dim))
    ang = jnp.arange(seq, dtype=f32)[:, None] * inv[None, :]
    return jnp.cos(ang), jnp.sin(ang)


def apply_rope(x, cos, sin):
    xf = x.astype(f32)
    x1, x2 = jnp.split(xf, 2, axis=-1)
    c = cos[None, :, None, :]
    s = sin[None, :, None, :]
    return jnp.concatenate([x1 * c - x2 * s, x2 * c + x1 * s], axis=-1).astype(x.dtype)


def dsa_attention(q, k, v, q_idx, k_idx, w_idx):
    B, S, H, Dh = q.shape
    topk = min(TOPK_MAX, S // 4)
    n_blocks = S // Q_BLOCK
    key_pos = jnp.arange(S)
    k_idx_f = k_idx.astype(f32)
    idx_scale = (IDX_HEADS ** -0.5) * (IDX_DIM ** -0.5)

    def block(i):
        start = i * Q_BLOCK
        qpos = start + jnp.arange(Q_BLOCK)
        qi = lax.dynamic_slice_in_dim(q_idx, start, Q_BLOCK, axis=1).astype(f32)
        wi = lax.dynamic_slice_in_dim(w_idx, start, Q_BLOCK, axis=1).astype(f32)
        logits = jnp.einsum('bqhd,bsd->bqhs', qi, k_idx_f)
        score = jnp.einsum('bqhs,bqh->bqs', jax.nn.relu(logits), wi) * idx_scale
        causal = key_pos[None, :] <= qpos[:, None]
        score = jnp.where(causal[None], score, -jnp.inf)
        _, sel = lax.top_k(score, topk)
        valid = sel <= qpos[None, :, None]
        k_sel = jax.vmap(lambda a, ib: a[ib])(k, sel).astype(f32)
        v_sel = jax.vmap(lambda a, ib: a[ib])(v, sel).astype(f32)
        qb = lax.dynamic_slice_in_dim(q, start, Q_BLOCK, axis=1).astype(f32)
        s = jnp.einsum('bqhd,bqkhd->bhqk', qb, k_sel) * (Dh ** -0.5)
        s = jnp.where(valid[:, None], s, NEG)
        p = jax.nn.softmax(s, axis=-1)
        o = jnp.einsum('bhqk,bqkhd->bqhd', p, v_sel)
        return o.astype(q.dtype)

    out = lax.map(block, jnp.arange(n_blocks))
    return out.transpose(1, 0, 2, 3, 4).reshape(B, S, H, Dh)


def diff_attention(q1, q2, k1, k2, v, lam):
    B, S, H, Dd = q1.shape
    n_blocks = S // Q_BLOCK
    key_pos = jnp.arange(S)
    k1f, k2f, vf = k1.astype(f32), k2.astype(f32), v.astype(f32)

    def block(i):
        start = i * Q_BLOCK
        qpos = start + jnp.arange(Q_BLOCK)
        causal = (key_pos[None, :] <= qpos[:, None])[None, None]

        def probs(qh, kf):
            qb = lax.dynamic_slice_in_dim(qh, start, Q_BLOCK, axis=1).astype(f32)
            s = jnp.einsum('bqhd,bshd->bhqs', qb, kf) * (Dd ** -0.5)
            return jax.nn.softmax(jnp.where(causal, s, NEG), axis=-1)

        p = probs(q1, k1f) - lam * probs(q2, k2f)
        o = jnp.einsum('bhqs,bshe->bqhe', p, vf)
        return o.astype(v.dtype)

    out = lax.map(block, jnp.arange(n_blocks))
    return out.transpose(1, 0, 2, 3, 4).reshape(B, S, H, 2 * Dd)


def setup_inputs(seed: int = 0) -> dict:
    key = jax.random.key(seed)
    ks = jax.random.split(key, 16)
    nrm = jax.random.normal
    x = nrm(ks[0], (BATCH, SEQ, D_MODEL), f32)
    w_in = nrm(ks[1], (DEPTH, D_MODEL, IN_COLS), f32) * D_MODEL ** -0.5
    w_out = nrm(ks[2], (DEPTH, MIX_W, D_MODEL), f32) * MIX_W ** -0.5
    w_up = nrm(ks[3], (DEPTH, D_MODEL, D_FF), f32) * D_MODEL ** -0.5
    w_down = nrm(ks[4], (DEPTH, D_FF, D_MODEL), f32) * D_FF ** -0.5
    g_pre_mix = 1.0 + 0.02 * nrm(ks[5], (DEPTH, D_MODEL), f32)
    g_post_mix = 1.0 + 0.02 * nrm(ks[6], (DEPTH, D_MODEL), f32)
    g_pre_mlp = 1.0 + 0.02 * nrm(ks[7], (DEPTH, D_MODEL), f32)
    g_post_mlp = 1.0 + 0.02 * nrm(ks[8], (DEPTH, D_MODEL), f32)
    g_diff_sub = 1.0 + 0.02 * nrm(ks[9], (DEPTH, 2 * DIFF_DIM), f32)
    lambda_q1 = 0.1 * nrm(ks[10], (DEPTH, DIFF_DIM), f32)
    lambda_k1 = 0.1 * nrm(ks[11], (DEPTH, DIFF_DIM), f32)
    lambda_q2 = 0.1 * nrm(ks[12], (DEPTH, DIFF_DIM), f32)
    lambda_k2 = 0.1 * nrm(ks[13], (DEPTH, DIFF_DIM), f32)
    return {"x": x, "w_in": w_in, "w_out": w_out, "w_up": w_up, "w_down": w_down,
            "g_pre_mix": g_pre_mix, "g_post_mix": g_post_mix,
            "g_pre_mlp": g_pre_mlp, "g_post_mlp": g_post_mlp,
            "g_diff_sub": g_diff_sub,
            "lambda_q1": lambda_q1, "lambda_k1": lambda_k1,
            "lambda_q2": lambda_q2, "lambda_k2": lambda_k2}


def reference(x, w_in, w_out, w_up, w_down, g_pre_mix, g_post_mix, g_pre_mlp,
              g_post_mlp, g_diff_sub, lambda_q1, lambda_k1, lambda_q2, lambda_k2):
    B, S, _ = x.shape
    cos, sin = rope_tables(S, HEAD_DIM)
    for layer in range(DEPTH):
        h = rmsnorm(x, g_pre_mix[layer])
        proj = jnp.einsum('bsd,dc->bsc', h, w_in[layer])
        (q_a, k_a, v_a, q_i, k_i, w_i, q_d, k_d, v_d) = jnp.split(proj, SPLIT_POINTS, axis=-1)

        q_a = apply_rope(q_a.reshape(B, S, DSA_HEADS, HEAD_DIM), cos, sin)
        k_a = apply_rope(k_a.reshape(B, S, DSA_HEADS, HEAD_DIM), cos, sin)
        v_a = v_a.reshape(B, S, DSA_HEADS, HEAD_DIM)
        q_i = apply_rope(q_i.reshape(B, S, IDX_HEADS, IDX_DIM), cos, sin)
        k_i = apply_rope(k_i.reshape(B, S, 1, IDX_DIM), cos, sin)[:, :, 0]
        o_a = dsa_attention(q_a, k_a, v_a, q_i, k_i, w_i)

        q_d = q_d.reshape(B, S, DIFF_HEADS, 2, DIFF_DIM)
        k_d = k_d.reshape(B, S, DIFF_HEADS, 2, DIFF_DIM)
        v_d = v_d.reshape(B, S, DIFF_HEADS, 2 * DIFF_DIM)
        q1 = apply_rope(q_d[:, :, :, 0], cos, sin)
        q2 = apply_rope(q_d[:, :, :, 1], cos, sin)
        k1 = apply_rope(k_d[:, :, :, 0], cos, sin)
        k2 = apply_rope(k_d[:, :, :, 1], cos, sin)
        lam_init = 0.8 - 0.6 * math.exp(-0.3 * layer)
        lam = (jnp.exp(jnp.sum(lambda_q1[layer].astype(f32) * lambda_k1[layer].astype(f32)))
               - jnp.exp(jnp.sum(lambda_q2[layer].astype(f32) * lambda_k2[layer].astype(f32)))
               + lam_init)
        o_d = diff_attention(q1, q2, k1, k2, v_d, lam)
        o_d = (rmsnorm(o_d, g_diff_sub[layer]).astype(f32) * (1.0 - lam_init)).astype(x.dtype)

        mix = jnp.concatenate([o_a.reshape(B, S, DSA_W), o_d.reshape(B, S, DIFF_W)], axis=-1)
        y = jnp.einsum('bsc,cd->bsd', mix, w_out[layer])
        x = x + rmsnorm(y, g_post_mix[layer])

        h = rmsnorm(x, g_pre_mlp[layer])
        u = jax.nn.relu(jnp.einsum('bsd,df->bsf', h, w_up[layer]))
        y = jnp.einsum('bsf,fd->bsd', u * u, w_down[layer])
        x = x + rmsnorm(y, g_post_mlp[layer])
    return x
```

```python
import math
import numpy as np
import ml_dtypes
import concourse.bass as bass
import concourse.mybir as mybir
from concourse.bass_utils import run_bass_kernel_spmd

F32 = mybir.dt.float32
BF16 = mybir.dt.bfloat16
ALU = mybir.AluOpType
AF = mybir.ActivationFunctionType
AX = mybir.AxisListType

S = 8192
D = 1024
DFF = 4096
NOWN = 2048
EPS = 1e-6
NEG = -30000.0
N_IT = 20
IDX_SCALE = (8 ** -0.5) * (64 ** -0.5)
LAM_INIT = 0.8 - 0.6 * math.exp(-0.3 * 0)

KA, KD, KI, VA, VD = 0, 512, 1024, 1152, 1664
WK_COLS = 2176
QA, QD, QI, WI = 0, 512, 1024, 1536
WQ_COLS = 1544


class Tok:
    __slots__ = ("w", "r", "psum")

    def __init__(self, psum=False):
        self.w = None
        self.r = []
        self.psum = psum


class Eng:
    def __init__(self, name, h, sem):
        self.name = name
        self.h = h
        self.sem = sem
        self.cnt = 0
        self.seen = {}


class KB:
    def __init__(self, nc, sems, ndma=24):
        self.nc = nc
        self.e = {
            "pe": Eng("pe", nc.tensor, sems[0]),
            "act": Eng("act", nc.scalar, sems[1]),
            "dve": Eng("dve", nc.vector, sems[2]),
            "pool": Eng("pool", nc.gpsimd, sems[3]),
            "sp": Eng("sp", nc.sync, None),
        }
        self.dsems = [[s, 0] for s in sems[4:4 + ndma]]
        self.drr = 0

    def _wait(self, eng, ev):
        sem, val = ev
        k = id(sem)
        if eng.seen.get(k, 0) >= val:
            return
        eng.h.wait_ge(sem, val)
        eng.seen[k] = val

    def _deps(self, eng, reads, writes):
        for t in reads:
            if t.w is not None:
                if not (eng.name == "pe" and t.w[0] is eng.sem):
                    self._wait(eng, t.w)
            if t.psum:
                for ev in t.r:
                    if ev[0] is not eng.sem:
                        self._wait(eng, ev)
        for t in writes:
            if t.w is not None:
                if not (eng.name == "pe" and t.w[0] is eng.sem):
                    self._wait(eng, t.w)
            for ev in t.r:
                if ev[0] is eng.sem and eng.name == "pe":
                    continue
                self._wait(eng, ev)

    def op(self, en, fn, reads=(), writes=()):
        eng = self.e[en]
        self._deps(eng, reads, writes)
        inst = fn(eng.h)
        eng.cnt += 1
        inst.then_inc(eng.sem, 1)
        ev = (eng.sem, eng.cnt)
        for t in reads:
            t.r.append(ev)
        for t in writes:
            t.w = ev
            t.r = []
        return inst

    def dma(self, out, in_, reads=(), writes=(), q="sp"):
        eng = self.e[q]
        ds = self.dsems[self.drr]
        self.drr = (self.drr + 1) % len(self.dsems)
        if ds[1] > 0:
            self._wait(eng, (ds[0], ds[1]))
        self._deps(eng, reads, writes)
        inst = eng.h.dma_start(out=out, in_=in_)
        ds[1] += 16
        inst.then_inc(ds[0], 16)
        ev = (ds[0], ds[1])
        for t in reads:
            t.r.append(ev)
        for t in writes:
            t.w = ev
            t.r = []
        return ev

    def barrier(self):
        names = ["pe", "act", "dve", "pool", "sp"]
        for a in names:
            ea = self.e[a]
            for b in names:
                eb = self.e[b]
                if a == b or eb.sem is None or eb.cnt == 0:
                    continue
                self._wait(ea, (eb.sem, eb.cnt))
            for ds in self.dsems:
                if ds[1] > 0:
                    self._wait(ea, (ds[0], ds[1]))


def toks(n):
    return [Tok() for _ in range(n)]


def build(debug=0):
    nc = bass.Bass("TRN2", target_bir_lowering=False)

    def din(name, shape, dt=F32):
        return nc.dram_tensor(name, list(shape), dt, kind="ExternalInput").ap()

    xT_d = din("xT", [D, S])
    xTo_d = din("xTo", [D, NOWN])
    xo_d = din("xo", [NOWN, D])
    wk_d = din("wk", [D, WK_COLS])
    wq_d = din("wq", [D, WQ_COLS])
    wout_d = din("wout", [D, D])
    wup_d = din("wup", [D, DFF])
    wdn_d = din("wdn", [DFF, D])
    gpm_d = din("gpm", [128, 8])
    gpl_d = din("gpl", [128, 8])
    gpost_d = din("gpost", [2, D])
    gds_d = din("gds", [128, 1])
    lam_d = din("lam", [4, 64])
    cos_d = din("cosf", [128, S])
    sin_d = din("sinf", [128, S])
    coso_d = din("coso", [128, NOWN])
    sino_d = din("sino", [128, NOWN])
    cm_d = din("cmask", [128, 4, 2048], BF16)
    id_d = din("ident", [128, 128], BF16)
    pm_d = din("permm", [128, 128], BF16)
    p2_d = din("pow2", [128, N_IT + 1])

    out_d = nc.dram_tensor("out", [NOWN, D], F32, kind="ExternalOutput").ap()
    kaT_s = nc.dram_tensor("kaT_s", [4, 128, S], BF16, kind="Internal").ap()
    kdT_s = nc.dram_tensor("kdT_s", [4, 128, S], BF16, kind="Internal").ap()
    va_s = nc.dram_tensor("va_s", [64, 128, 512], BF16, kind="Internal").ap()
    vd_s = nc.dram_tensor("vd_s", [64, 128, 512], BF16, kind="Internal").ap()
    q_s = nc.dram_tensor("q_s", [3, 4, 128, NOWN], BF16, kind="Internal").ap()
    mix_s = nc.dram_tensor("mix_s", [8, 128, NOWN], BF16, kind="Internal").ap()
    wo_b = nc.dram_tensor("wo_b", [8, 128, D], BF16, kind="Internal").ap()
    wu_b = nc.dram_tensor("wu_b", [8, 128, DFF], BF16, kind="Internal").ap()
    wd_b = nc.dram_tensor("wd_b", [32, 128, D], BF16, kind="Internal").ap()
    dbg = {}
    if debug:
        dbg["kiT"] = nc.dram_tensor("dbg_kiT", [128, S], BF16, kind="ExternalOutput").ap()
        dbg["kaT"] = nc.dram_tensor("dbg_kaT", [4, 128, S], BF16, kind="ExternalOutput").ap()
        dbg["va"] = nc.dram_tensor("dbg_va", [64, 128, 512], BF16, kind="ExternalOutput").ap()
        dbg["qaT"] = nc.dram_tensor("dbg_qaT", [128, 4, NOWN], BF16, kind="ExternalOutput").ap()
        dbg["wtok"] = nc.dram_tensor("dbg_wtok", [128, 16, 8], F32, kind="ExternalOutput").ap()
        dbg["mixT"] = nc.dram_tensor("dbg_mixT", [128, 8, NOWN], BF16, kind="ExternalOutput").ap()
        dbg["thr"] = nc.dram_tensor("dbg_thr", [128, 16], F32, kind="ExternalOutput").ap()

    from contextlib import ExitStack
    es = ExitStack()
    with es:
        sems = [es.enter_context(nc.semaphore(f"s{i}")) for i in range(4 + 24)]
        kb = KB(nc, sems)
        op, dma = kb.op, kb.dma

        def sb(name, shape, dt, stack=es):
            return stack.enter_context(nc.sbuf_tensor(name, list(shape), dt))

        ps = [es.enter_context(nc.psum_tensor(f"ps{i}", [128, 512], F32)) for i in range(8)]
        pst = [Tok(psum=True) for _ in range(8)]

        ident = sb("ident_sb", [128, 128], BF16); t_ident = Tok()
        ones_bf = sb("ones_bf", [128, 128], BF16); t_ones = Tok()
        neglam = sb("neglam", [128, 1], F32); t_lam = Tok()
        gds = sb("gds_sb", [128, 1], F32); t_gds = Tok()
        pow2 = sb("pow2_sb", [128, N_IT + 1], F32); t_p2 = Tok()
        epst = sb("epst", [128, 1], F32); t_eps = Tok()
        t_mix = [[Tok() for _ in range(4)] for _ in range(8)]

        dma(ident[:], id_d[:, :], writes=[t_ident])
        permt = sb("permt", [128, 128], BF16); t_perm = Tok()
        dma(permt[:], pm_d[:, :], writes=[t_perm])
        dma(pow2[:], p2_d[:, :], writes=[t_p2])
        op("pool", lambda e: e.memset(ones_bf[:], 1.0), writes=[t_ones])
        op("pool", lambda e: e.memset(epst[:], EPS), writes=[t_eps])

        with ExitStack() as s0:
            lamt = sb("lamt", [128, 4, 64], F32, s0); t_lamt = Tok()
            lprod = sb("lprod", [128, 2, 64], F32, s0); t_lp = Tok()
            lsum = sb("lsum", [128, 2], F32, s0); t_ls = Tok()
            lexp = sb("lexp", [128, 2], F32, s0); t_le = Tok()
            for r in range(4):
                dma(lamt[:, r, :], lam_d[r:r + 1, :].partition_broadcast(128), writes=[t_lamt])
            op("dve", lambda e: e.tensor_tensor(out=lprod[:, 0, :], in0=lamt[:, 0, :], in1=lamt[:, 1, :], op=ALU.mult),
               reads=[t_lamt], writes=[t_lp])
            op("dve", lambda e: e.tensor_tensor(out=lprod[:, 1, :], in0=lamt[:, 2, :], in1=lamt[:, 3, :], op=ALU.mult),
               reads=[t_lamt], writes=[t_lp])
            op("dve", lambda e: e.tensor_reduce(out=lsum[:, :], in_=lprod[:, :, :], axis=AX.X, op=ALU.add),
               reads=[t_lp], writes=[t_ls])
            op("act", lambda e: e.activation(out=lexp[:, :], in_=lsum[:, :], func=AF.Exp), reads=[t_ls], writes=[t_le])
            op("dve", lambda e: e.tensor_tensor(out=neglam[:, :], in0=lexp[:, 1:2], in1=lexp[:, 0:1], op=ALU.subtract),
               reads=[t_le], writes=[t_lam])
            op("dve", lambda e: e.tensor_scalar(out=neglam[:, :], in0=neglam[:, :], scalar1=-LAM_INIT, scalar2=None, op0=ALU.add),
               reads=[t_lam], writes=[t_lam])
            dma(gds[:], gds_d[:, :], writes=[t_gds])
            op("dve", lambda e: e.tensor_scalar(out=gds[:, :], in0=gds[:, :], scalar1=1.0 - LAM_INIT, scalar2=None, op0=ALU.mult),
               reads=[t_gds], writes=[t_gds])
            kb.barrier()

        s12 = es.enter_context(ExitStack())
        kiT = sb("kiT", [128, S], BF16, s12); t_ki = toks(16)
        t_qs = [[[Tok() for _ in range(4)] for _ in range(4)] for _ in range(3)]
        wtok = sb("wtok", [128, 16, 8], F32, s12); t_wtok = toks(16)
        t_kas = [[Tok() for _ in range(16)] for _ in range(4)]
        t_kds = [[Tok() for _ in range(16)] for _ in range(4)]
        t_vas = toks(64)
        t_vds = toks(64)
        t_wob = toks(8); t_wub = toks(8); t_wdb = toks(32)

        def phase1():
            with ExitStack() as s1:
                sfx = "_p1"
                gpm = sb("gpm_sb" + sfx, [128, 8], F32, s1); t_gpm = Tok()
                dma(gpm[:], gpm_d[:, :], writes=[t_gpm])
                Wk = sb("Wk", [128, 8, WK_COLS], BF16, s1); twk = toks(8)
                Wq = sb("Wq", [128, 8, WQ_COLS], BF16, s1); twq = toks(8)
                NW1 = 4
                wst = [sb(f"wst{i}" + sfx, [128, 1024], F32, s1) for i in range(NW1)]; t_wst = toks(NW1)
                n = 0
                bg = []

                def conv_job(w_d, Wb, twb, ncol, t, c0):
                    nonlocal n
                    w_v = w_d.rearrange("(t p) c -> t p c", p=128)
                    c1 = min(ncol, c0 + 1024)
                    bq = n % NW1
                    dma(wst[bq][:, :c1 - c0], w_v[t, :, c0:c1], writes=[t_wst[bq]])
                    en = ("dve", "pool", "act")[n % 3]
                    if en == "act":
                        op(en, lambda e: e.activation(out=Wb[:, t, c0:c1], in_=wst[bq][:, :c1 - c0], func=AF.Copy,
                                                      scale=gpm[:, t:t + 1]),
                           reads=[t_wst[bq], t_gpm], writes=[twb[t]])
                    else:
                        op(en, lambda e: e.tensor_scalar(
                            out=Wb[:, t, c0:c1], in0=wst[bq][:, :c1 - c0], scalar1=gpm[:, t:t + 1], scalar2=None,
                            op0=ALU.mult), reads=[t_wst[bq], t_gpm], writes=[twb[t]])
                    n += 1

                for t in range(8):
                    for c0 in range(0, WK_COLS, 1024):
                        conv_job(wk_d, Wk, twk, WK_COLS, t, c0)
                for t in range(8):
                    for c0 in range(0, WQ_COLS, 1024):
                        bg.append(lambda t=t, c0=c0: conv_job(wq_d, Wq, twq, WQ_COLS, t, c0))

                gpl1 = sb("gpl_p1", [128, 8], F32, s1); t_gpl1 = Tok()
                dma(gpl1[:], gpl_d[:, :], writes=[t_gpl1])
                NB3 = 3
                w3f = [sb(f"w3f{i}", [128, 1024], F32, s1) for i in range(NB3)]; t_w3f = toks(NB3)
                w3b = [sb(f"w3b{i}", [128, 1024], BF16, s1) for i in range(NB3)]; t_w3b = toks(NB3)
                n3 = [0]

                def w3_job(src, dst, tk, gt):
                    k = n3[0] % NB3
                    dma(w3f[k][:], src, writes=[t_w3f[k]])
                    en = ("pool", "act")[n3[0] % 2]
                    if gt is None:
                        if en == "act":
                            op(en, lambda e: e.copy(out=w3b[k][:], in_=w3f[k][:]), reads=[t_w3f[k]], writes=[t_w3b[k]])
                        else:
                            op(en, lambda e: e.tensor_copy(out=w3b[k][:], in_=w3f[k][:]), reads=[t_w3f[k]], writes=[t_w3b[k]])
                    else:
                        if en == "act":
                            op(en, lambda e: e.activation(out=w3b[k][:], in_=w3f[k][:], func=AF.Copy,
                                                          scale=gpl1[:, gt:gt + 1]),
                               reads=[t_w3f[k], t_gpl1], writes=[t_w3b[k]])
                        else:
                            op(en, lambda e: e.tensor_scalar(out=w3b[k][:], in0=w3f[k][:], scalar1=gpl1[:, gt:gt + 1],
                                                             scalar2=None, op0=ALU.mult),
                               reads=[t_w3f[k], t_gpl1], writes=[t_w3b[k]])
                    dma(dst, w3b[k][:], reads=[t_w3b[k]], writes=[tk])
                    n3[0] += 1

                wo_v = wout_d.rearrange("(t p) c -> t p c", p=128)
                wu_v = wup_d.rearrange("(t p) c -> t p c", p=128)
                wd_v = wdn_d.rearrange("(t p) c -> t p c", p=128)
                bg3 = [(lambda t=t: w3_job(wo_v[t, :, :], wo_b[t, :, :], t_wob[t], None)) for t in range(8)]
                bg3 += [(lambda t=t, c0=c0: w3_job(wu_v[t, :, c0:c0 + 1024], wu_b[t, :, c0:c0 + 1024], t_wub[t], t))
                        for t in range(8) for c0 in range(0, DFF, 1024)]
                bg3 += [(lambda t=t: w3_job(wd_v[t, :, :], wd_b[t, :, :], t_wdb[t], None)) for t in range(32)]

                def run_bg(nq, n3j):
                    for _ in range(nq):
                        if bg:
                            bg.pop(0)()
                    for _ in range(n3j):
                        if bg3:
                            bg3.pop(0)()

                xc = [sb(f"xc{i}" + sfx, [128, 8, 512], F32, s1) for i in range(2)]; t_xc = toks(2)
                sq = sb("sq" + sfx, [128, 8, 512], BF16, s1); t_sq = Tok()
                rs1 = sb("rs1" + sfx, [128, 512], F32, s1); t_rs1 = Tok()
                rstd = sb("rstd" + sfx, [128, 512], F32, s1); t_rstd = Tok()
                hT = [sb(f"hT{i}" + sfx, [128, 8, 512], BF16, s1) for i in range(2)]; t_hT = [toks(8), toks(8)]
                cst = [sb(f"cst{i}" + sfx, [128, 512], F32, s1) for i in range(2)]; t_cst = toks(2)
                sst = [sb(f"sst{i}" + sfx, [128, 512], F32, s1) for i in range(2)]; t_sst = toks(2)
                r1 = [sb(f"r1_{i}" + sfx, [128, 512], F32, s1) for i in range(2)]; t_r1 = toks(2)
                r2 = [sb(f"r2_{i}" + sfx, [128, 512], F32, s1) for i in range(2)]; t_r2 = toks(2)
                kst = [sb(f"kst{i}" + sfx, [128, 512], BF16, s1) for i in range(4)]; t_kst = toks(4)
                vst = [sb(f"vst{i}" + sfx, [128, 512], BF16, s1) for i in range(4)]; t_vst = toks(4)
                pbf = [sb(f"pbf{i}" + sfx, [128, 512], BF16, s1) for i in range(2)]; t_pbf = toks(2)

                xT_v = xT_d.rearrange("(t p) n -> p t n", p=128)
                xTo_v = xTo_d.rearrange("(t p) n -> p t n", p=128)
                cnt = {"rope": 0, "v": 0, "pair": 0, "k": 0, "ci": 0}
                pend = []

                def norm_chunk(xsrc, csrc, ssrc, c):
                    b = cnt["ci"] % 2
                    cnt["ci"] += 1
                    dma(xc[b][:], xsrc[:, :, c * 512:(c + 1) * 512], writes=[t_xc[b]])
                    dma(cst[b][:], csrc[:, c * 512:(c + 1) * 512], writes=[t_cst[b]])
                    dma(sst[b][:], ssrc[:, c * 512:(c + 1) * 512], writes=[t_sst[b]])
                    op("act", lambda e: e.activation(out=sq[:], in_=xc[b][:], func=AF.Square),
                       reads=[t_xc[b]], writes=[t_sq])
                    for t in range(8):
                        op("pe", lambda e: e.matmul(ps[7][:, :], lhsT=ones_bf[:, :], rhs=sq[:, t, :],
                                                    start=(t == 0), stop=(t == 7)),
                           reads=[t_ones, t_sq], writes=[pst[7]])
                    op("dve", lambda e: e.tensor_scalar(out=rs1[:], in0=ps[7][:, :], scalar1=1.0 / D, scalar2=EPS,
                                                        op0=ALU.mult, op1=ALU.add), reads=[pst[7]], writes=[t_rs1])
                    op("act", lambda e: e.activation(out=rs1[:], in_=rs1[:], func=AF.Ln), reads=[t_rs1], writes=[t_rs1])
                    op("act", lambda e: e.activation(out=rstd[:], in_=rs1[:], func=AF.Exp, scale=-0.5),
                       reads=[t_rs1], writes=[t_rstd])
                    for t in range(8):
                        en = "dve" if t % 2 == 0 else "pool"
                        op(en, lambda e: e.tensor_tensor(out=hT[b][:, t, :], in0=xc[b][:, t, :], in1=rstd[:],
                                                         op=ALU.mult),
                           reads=[t_xc[b], t_rstd], writes=[t_hT[b][t]])
                    return b

                def rope_finish(item):
                    (b, pa, pb, k, dst_ap, wtoks, after) = item
                    op("pe", lambda e: e.matmul(ps[pb][:, :], lhsT=permt[:, :], rhs=pbf[k][:], start=True, stop=True),
                       reads=[t_perm, t_pbf[k]], writes=[pst[pb]])
                    op("dve", lambda e: e.tensor_tensor(out=r1[k][:], in0=ps[pa][:, :], in1=cst[b][:], op=ALU.mult),
                       reads=[pst[pa], t_cst[b]], writes=[t_r1[k]])
                    op("dve", lambda e: e.tensor_tensor(out=r2[k][:], in0=ps[pb][:, :], in1=sst[b][:], op=ALU.mult),
                       reads=[pst[pb], t_sst[b]], writes=[t_r2[k]])
                    op("pool", lambda e: e.tensor_tensor(out=dst_ap, in0=r1[k][:], in1=r2[k][:], op=ALU.add),
                       reads=[t_r1[k], t_r2[k]], writes=wtoks)
                    if after is not None:
                        after()

                def rope_tile(b, W, tw, c0, dst_ap, wtoks, after=None):
                    pp = cnt["pair"] % 2
                    cnt["pair"] += 1
                    pa, pb = 2 * pp, 2 * pp + 1
                    for t in range(8):
                        op("pe", lambda e: e.matmul(
                            ps[pa][:, :], lhsT=W[:, t, c0:c0 + 128], rhs=hT[b][:, t, :],
                            start=(t == 0), stop=(t == 7)),
                           reads=[tw[t], t_hT[b][t]], writes=[pst[pa]])
                    k = cnt["rope"] % 2
                    cnt["rope"] += 1
                    op("act", lambda e: e.copy(out=pbf[k][:], in_=ps[pa][:, :]), reads=[pst[pa]], writes=[t_pbf[k]])
                    if pend:
                        rope_finish(pend.pop(0))
                    pend.append((b, pa, pb, k, dst_ap, wtoks, after))

                def rope_flush():
                    while pend:
                        rope_finish(pend.pop(0))

                for c in range(16):
                    b = norm_chunk(xT_v, cos_d, sin_d, c)
                    for (base, spill, tsp) in ((KA, kaT_s, t_kas), (KD, kdT_s, t_kds)):
                        for i in range(4):
                            k = cnt["k"] % 4
                            cnt["k"] += 1

                            def after(k=k, spill=spill, i=i, c=c, tsp=tsp):
                                dma(spill[i, :, c * 512:(c + 1) * 512], kst[k][:], reads=[t_kst[k]],
                                    writes=[tsp[i][c]])
                            rope_tile(b, Wk, twk, base + 128 * i, kst[k][:], [t_kst[k]], after)
                    rope_tile(b, Wk, twk, KI, kiT[:, c * 512:(c + 1) * 512], [t_ki[c]])
                    rope_flush()
                    for st in range(4):
                        for (base, spill, tsp) in ((VA, va_s, t_vas), (VD, vd_s, t_vds)):
                            bank = 4 + cnt["v"] % 2
                            k = cnt["v"] % 4
                            cnt["v"] += 1
                            for t in range(8):
                                op("pe", lambda e: e.matmul(
                                    ps[bank][:, :], lhsT=hT[b][:, t, st * 128:(st + 1) * 128],
                                    rhs=Wk[:, t, base:base + 512], start=(t == 0), stop=(t == 7)),
                                   reads=[twk[t], t_hT[b][t]], writes=[pst[bank]])
                            op("act", lambda e: e.copy(out=vst[k][:], in_=ps[bank][:, :]),
                               reads=[pst[bank]], writes=[t_vst[k]])
                            dma(spill[4 * c + st, :, :], vst[k][:], reads=[t_vst[k]], writes=[tsp[4 * c + st]])
                    run_bg(1, 4)
                run_bg(99, 0)
                for c in range(4):
                    b = norm_chunk(xTo_v, coso_d, sino_d, c)
                    for (wh, base) in ((0, QA), (1, QD), (2, QI)):
                        for i in range(4):
                            k = cnt["k"] % 4
                            cnt["k"] += 1

                            def after(k=k, wh=wh, i=i, c=c):
                                dma(q_s[wh, i, :, c * 512:(c + 1) * 512], kst[k][:], reads=[t_kst[k]],
                                    writes=[t_qs[wh][i][c]])
                            rope_tile(b, Wq, twq, base + 128 * i, kst[k][:], [t_kst[k]], after)
                    rope_flush()
                    for st in range(4):
                        g = 4 * c + st
                        bank = 4 + cnt["v"] % 2
                        cnt["v"] += 1
                        for t in range(8):
                            op("pe", lambda e: e.matmul(
                                ps[bank][:, 0:8], lhsT=hT[b][:, t, st * 128:(st + 1) * 128],
                                rhs=Wq[:, t, WI:WI + 8], start=(t == 0), stop=(t == 7)),
                               reads=[twq[t], t_hT[b][t]], writes=[pst[bank]])
                        op("dve", lambda e: e.tensor_scalar(
                            out=wtok[:, g, :], in0=ps[bank][:, 0:8], scalar1=IDX_SCALE, scalar2=None,
                            op0=ALU.mult), reads=[pst[bank]], writes=[t_wtok[g]])
                    run_bg(0, 2)
                run_bg(0, 999)
                kb.barrier()

        phase1()

        if debug == 1:
            dma(dbg["kiT"][:, :], kiT[:], reads=t_ki)
            dma(dbg["wtok"][:, :, :], wtok[:], reads=t_wtok)
            for i in range(4):
                dma(dbg["qaT"][:, i, :], q_s[0, i, :, :], reads=t_qs[0][i])
                dma(dbg["kaT"][i, :, :], kaT_s[i, :, :], reads=t_kas[i])
            dma(dbg["va"][:, :, :], va_s[:, :, :], reads=t_vas)
            kb.barrier()
            return nc

        def phase2(slots):
            F8 = mybir.dt.float8e5
            with ExitStack() as s2:
                cmask = sb("cmask8", [128, 4, 2048], F8, s2); t_cm = Tok()
                id8 = sb("id8", [128, 128], F8, s2); t_id8 = Tok()
                with ExitStack() as s2t:
                    cmt = sb("cmask_tmp", [128, 4, 2048], BF16, s2t); t_cmt = Tok()
                    dma(cmt[:], cm_d[:, :, :], writes=[t_cmt])
                    op("dve", lambda e: e.tensor_copy(out=cmask[:], in_=cmt[:], saturate=False),
                       reads=[t_cmt], writes=[t_cm])
                    op("dve", lambda e: e.tensor_copy(out=id8[:], in_=ident[:], saturate=False),
                       reads=[t_ident], writes=[t_id8])
                    kb.barrier()
                QbI = sb("QbI", [128, 8, 512], BF16, s2); t_QbI = Tok()
                QbA = sb("QbA", [128, 8, 512], BF16, s2); t_QbA = Tok()
                QbD = QbA; t_QbD = t_QbA
                for (qb_, tq_) in ((QbI, t_QbI), (QbA, t_QbA)):
                    op("pool", lambda e: e.memset(qb_[:], 0.0), writes=[tq_])

                def load_qpad(qb_, tq_, wh, blk):
                    for t in range(4):
                        for half in range(2):
                            dma(qb_[64 * half:64 * half + 64, 2 * t + half, :],
                                q_s[wh, t, 64 * half:64 * half + 64, blk], reads=t_qs[wh][t], writes=[tq_])
                scores = [sb(f"score{k}", [128, S], F32, s2) for k in range(2)]; t_scs = [toks(16), toks(16)]
                mbr = sb("mbr", [128, 6, S], F8, s2)
                t_mbr = [toks(16) for _ in range(6)]
                msA = [sb(f"msA{k}", [128, 512], BF16, s2) for k in range(2)]; t_msA = toks(2)
                msD = [sb(f"msD{k}", [128, 512], BF16, s2) for k in range(2)]; t_msD = toks(2)
                Dm = [sb(f"Dm{i}", [128, 8, 128], BF16, s2) for i in range(2)]; t_Dm = toks(2)
                Rb = [sb(f"Rb{i}", [128, 512], BF16, s2) for i in range(4)]; t_Rb = toks(4)
                am = sb("am", [128, 2], F32, s2); t_am = Tok()
                steps = sb("steps", [128, N_IT + 1], F32, s2); t_steps = Tok()
                thrv = sb("thrv", [128, 2], F32, s2); t_thr = toks(2)
                thrf = sb("thrf", [128, 16], F32, s2); t_thrf = toks(16)
                cntv = sb("cntv", [128, 1], F32, s2); t_cnt = Tok()
                dv = sb("dv", [128, 1], F32, s2); t_dv = Tok()
                kck = [sb(f"kck{i}", [128, 512], BF16, s2) for i in range(2)]; t_kck = toks(2)
                kcv = [sb(f"kcv{i}", [128, 4, 128], BF16, s2) for i in range(2)]; t_kcv = toks(2)
                kva = [sb(f"kva{i}", [128, 4, 2, 128], BF16, s2) for i in range(2)]; t_kva = toks(2)
                stO = [sb(f"stO{i}", [128, 512], F32, s2) for i in range(2)]; t_stO = toks(2)
                stR = [sb(f"stR{i}", [64, 512], F32, s2) for i in range(2)]; t_stR = toks(2)
                Pb = [sb(f"Pb{i}", [128, 512], BF16, s2) for i in range(4)]; t_Pb = toks(4)
                kdk = [sb(f"kdk{i}", [128, 512], BF16, s2) for i in range(2)]; t_kdk = toks(2)
                kdv = [sb(f"kdv{i}", [128, 4, 128], BF16, s2) for i in range(2)]; t_kdv = toks(2)
                _stg = [sb(f"stg_{k}", [128, 512], F32, s2) for k in range(4)]
                stg = [_stg, _stg]
                _tstg = toks(4)
                t_stg = [_tstg, _tstg]
                _sqb = sb("sqb", [128, 512], BF16, s2); _tsqb = Tok()
                sqb = [_sqb, _sqb]; t_sqb = [_tsqb, _tsqb]
                for i in range(2):
                    op("pool", lambda e: e.memset(kva[i][:], 1.0), writes=[t_kva[i]])
                cc = {"ld": 0, "ld2": 0, "s": 0, "dm": 0}

                def build_Dm(g):
                    k = cc["dm"] % 2
                    cc["dm"] += 1
                    op("dve", lambda e: e.tensor_tensor(
                        out=Dm[k][:], in0=ident[:].unsqueeze(1).to_broadcast([128, 8, 128]),
                        in1=wtok[:, g, :].unsqueeze(2).to_broadcast([128, 8, 128]), op=ALU.mult),
                       reads=[t_ident, t_wtok[g]], writes=[t_Dm[k]])
                    return k

                def gen_2a(i):
                    nk = 2048 * (i + 1)
                    nch = 4 * (i + 1)
                    blk = slice(i * 512, (i + 1) * 512)
                    load_qpad(QbI, t_QbI, 2, blk)
                    dk = build_Dm(4 * i)
                    for qt in range(4):
                        g = 4 * i + qt
                        njobs = 8 * nch
                        score = scores[g % 2]
                        t_sc = t_scs[g % 2]
                        reg = g % 6

                        def emitD(j):
                            kc, h = divmod(j, 8)
                            sbank = 6 + kc % 2
                            op("pe", lambda e: e.matmul(ps[sbank][:, :], lhsT=Dm[dk][:, h, :], rhs=Rb[j % 4][:],
                                                        start=(h == 0), stop=(h == 7)),
                               reads=[t_Dm[dk], t_Rb[j % 4]], writes=[pst[sbank]])
                            if h == 7:
                                sl = slice(kc * 512, (kc + 1) * 512)
                                op("act", lambda e: e.copy(out=score[:, sl], in_=ps[sbank][:, :]),
                                   reads=[pst[sbank]], writes=[t_sc[kc]])

                        for j in range(njobs):
                            kc, h = divmod(j, 8)
                            pb_ = 64 * (h % 2)
                            bank = 4 + j % 2
                            op("pe", lambda e: e.matmul(
                                ps[bank][:, :], lhsT=QbI[:, h, qt * 128:(qt + 1) * 128],
                                rhs=kiT[:, kc * 512:(kc + 1) * 512], start=True, stop=True),
                               reads=[t_QbI, t_ki[kc]], writes=[pst[bank]])
                            op("act", lambda e: e.activation(out=Rb[j % 4][:], in_=ps[bank][:, :], func=AF.Relu),
                               reads=[pst[bank]], writes=[t_Rb[j % 4]])
                            if j >= 1:
                                emitD(j - 1)
                            if h == 7 and kc % 2 == 1:
                                yield "s"
                        emitD(njobs - 1)
                        yield "scored"
                        dk_next = build_Dm(g + 1) if qt < 3 else None
                        op("dve", lambda e: e.tensor_reduce(
                            out=am[:, 0:1], in_=score[:, 0:nk], axis=AX.X, op=ALU.max,
                            apply_absolute_value=True), reads=t_sc[:nch], writes=[t_am])
                        for r in range(4):
                            kc = nch - 4 + r
                            op("dve", lambda e: e.tensor_tensor(
                                out=score[:, kc * 512:(kc + 1) * 512], in0=score[:, kc * 512:(kc + 1) * 512],
                                in1=cmask[:, qt, r * 512:(r + 1) * 512], op=ALU.add),
                               reads=[t_cm, t_sc[kc]], writes=[t_sc[kc]])
                        op("dve", lambda e: e.tensor_scalar(out=am[:, 1:2], in0=am[:, 0:1], scalar1=1.001,
                                                            scalar2=1e-20, op0=ALU.mult, op1=ALU.add),
                           reads=[t_am], writes=[t_am])
                        op("dve", lambda e: e.tensor_scalar(out=steps[:], in0=pow2[:], scalar1=am[:, 1:2],
                                                            scalar2=None, op0=ALU.mult),
                           reads=[t_am, t_p2], writes=[t_steps])
                        op("dve", lambda e: e.memset(thrv[:, 0:1], 0.0), writes=[t_thr[0]])
                        for it in range(N_IT):
                            cur, nxt = it % 2, (it + 1) % 2
                            op("dve", lambda e: e.tensor_scalar(
                                out=mbr[:, reg, 0:nk], in0=score[:, 0:nk], scalar1=thrv[:, cur:cur + 1], scalar2=None,
                                op0=ALU.is_ge, op1=ALU.add, accum_out=cntv[:, 0:1], saturate=False),
                               reads=t_sc[:nch] + [t_thr[cur]], writes=t_mbr[reg][:nch] + [t_cnt])
                            op("dve", lambda e: e.tensor_scalar(out=dv[:], in0=cntv[:], scalar1=255.5, scalar2=0.5,
                                                                op0=ALU.is_ge, op1=ALU.subtract),
                               reads=[t_cnt], writes=[t_dv])
                            op("dve", lambda e: e.scalar_tensor_tensor(
                                out=thrv[:, nxt:nxt + 1], in0=dv[:], scalar=steps[:, it:it + 1], in1=thrv[:, cur:cur + 1],
                                op0=ALU.mult, op1=ALU.add), reads=[t_dv, t_steps, t_thr[cur]], writes=[t_thr[nxt]])
                        fin = N_IT % 2
                        op("dve", lambda e: e.scalar_tensor_tensor(
                            out=thrf[:, g:g + 1], in0=steps[:, N_IT:N_IT + 1], scalar=-1.0, in1=thrv[:, fin:fin + 1],
                            op0=ALU.mult, op1=ALU.add), reads=[t_steps, t_thr[fin]], writes=[t_thrf[g]])
                        op("dve", lambda e: e.tensor_scalar(
                            out=mbr[:, reg, 0:nk], in0=score[:, 0:nk], scalar1=thrf[:, g:g + 1], scalar2=NEG,
                            op0=ALU.is_lt, op1=ALU.mult, saturate=False),
                           reads=t_sc[:nch] + [t_thrf[g]], writes=t_mbr[reg][:nch])
                        dk = dk_next
                        yield "bisected"

                def gen_2b(i):
                    nch = 4 * (i + 1)
                    blk = slice(i * 512, (i + 1) * 512)
                    load_qpad(QbA, t_QbA, 0, blk)
                    for p in range(4):
                        njobs = nch * 8
                        bufs = {}

                        def load_dsa(kc):
                            bb = cc["ld"] % 2
                            cc["ld"] += 1
                            dma(kck[bb][:], kaT_s[p, :, kc * 512:(kc + 1) * 512], reads=[t_kas[p][kc]], writes=[t_kck[bb]])
                            dma(kcv[bb][:], va_s[4 * kc:4 * kc + 4, :, 128 * p:128 * p + 128].rearrange("t p c -> p t c"),
                                reads=t_vas[4 * kc:4 * kc + 4], writes=[t_kcv[bb]])
                            op("pool", lambda e: e.tensor_copy(
                                out=kva[bb][:, :, :, 0:64], in_=kcv[bb][:].rearrange("p t (h d) -> p t h d", h=2)),
                               reads=[t_kcv[bb]], writes=[t_kva[bb]])
                            bufs[kc] = bb

                        def emitPV(j):
                            kc, rem = divmod(j, 8)
                            kt, hh = divmod(rem, 2)
                            bb = bufs[kc]
                            pbuf = j % 4
                            op("pe", lambda e: e.matmul(ps[hh][:, :], lhsT=kva[bb][:, kt, hh, :], rhs=Pb[pbuf][:],
                                                        start=(kc == 0 and kt == 0), stop=(kc == nch - 1 and kt == 3)),
                               reads=[t_kva[bb], t_Pb[pbuf]], writes=[pst[hh]])

                        load_dsa(0)
                        for j in range(njobs):
                            kc, rem = divmod(j, 8)
                            kt, hh = divmod(rem, 2)
                            if rem == 2 and kc + 1 < nch:
                                load_dsa(kc + 1)
                            bb = bufs[kc]
                            sbank = 2 + cc["s"] % 2
                            cc["s"] += 1
                            op("pe", lambda e: e.matmul(
                                ps[sbank][:, :], lhsT=kck[bb][:, kt * 128:(kt + 1) * 128],
                                rhs=QbA[:, 2 * p + hh, :], start=True, stop=False),
                               reads=[t_kck[bb], t_QbA], writes=[pst[sbank]])
                            s0 = kc * 512 + kt * 128
                            for qt in range(4):
                                reg = (4 * i + qt) % 6
                                op("pe", lambda e: e.matmul(
                                    ps[sbank][:, qt * 128:(qt + 1) * 128], lhsT=mbr[:, reg, s0:s0 + 128], rhs=id8[:],
                                    start=False, stop=(qt == 3)),
                                   reads=[t_mbr[reg][kc], t_id8], writes=[pst[sbank]])
                            pbuf = j % 4
                            op("act", lambda e: e.activation(out=Pb[pbuf][:], in_=ps[sbank][:, :], func=AF.Exp,
                                                             scale=0.125), reads=[pst[sbank]], writes=[t_Pb[pbuf]])
                            if j >= 2:
                                emitPV(j - 2)
                            yield "b"
                        for j in range(njobs - 2, njobs):
                            emitPV(j)
                        k = p % 2
                        for hh in range(2):
                            op("act", lambda e: e.copy(out=stO[hh][0:64, :], in_=ps[hh][0:64, :]),
                               reads=[pst[hh]], writes=[t_stO[hh]])
                            op("act", lambda e: e.activation(out=stO[hh][64:128, :], in_=ps[hh][64:128, :], func=AF.Ln),
                               reads=[pst[hh]], writes=[t_stO[hh]])
                            op("act", lambda e: e.activation(out=stR[hh][0:64, :], in_=stO[hh][64:128, :],
                                                             func=AF.Exp, scale=-1.0),
                               reads=[t_stO[hh]], writes=[t_stR[hh]])
                            op("pool", lambda e: e.tensor_tensor(
                                out=msA[k][64 * hh:64 * hh + 64, :], in0=stO[hh][0:64, :], in1=stR[hh][0:64, :],
                                op=ALU.mult), reads=[t_stO[hh], t_stR[hh]], writes=[t_msA[k]])
                        dma(mix_s[p, :, blk], msA[k][:], reads=[t_msA[k]], writes=[t_mix[p][i]])
                    yield "bdone"

                def gen_2c(i):
                    nch = 4 * (i + 1)
                    blk = slice(i * 512, (i + 1) * 512)
                    load_qpad(QbD, t_QbD, 1, blk)
                    pending = []

                    def fin_stage3(h):
                        a = h % 2
                        sbank = 2 + cc["s"] % 2
                        cc["s"] += 1
                        op("act", lambda e: e.activation(out=sqb[a][:], in_=stg[a][0][:], func=AF.Square),
                           reads=[t_stg[a][0]], writes=[t_sqb[a]])
                        op("pe", lambda e: e.matmul(ps[sbank][:, :], lhsT=ones_bf[:], rhs=sqb[a][:], start=True, stop=True),
                           reads=[t_ones, t_sqb[a]], writes=[pst[sbank]])
                        op("act", lambda e: e.activation(out=stg[a][1][:], in_=ps[sbank][:, :], func=AF.Ln,
                                                         scale=1.0 / 128, bias=epst[:, 0:1]),
                           reads=[pst[sbank], t_eps], writes=[t_stg[a][1]])
                        op("act", lambda e: e.activation(out=stg[a][1][:], in_=stg[a][1][:], func=AF.Exp, scale=-0.5),
                           reads=[t_stg[a][1]], writes=[t_stg[a][1]])
                        op("pool", lambda e: e.tensor_scalar(out=stg[a][0][:], in0=stg[a][0][:], scalar1=gds[:, 0:1],
                                                             scalar2=None, op0=ALU.mult),
                           reads=[t_stg[a][0], t_gds], writes=[t_stg[a][0]])
                        op("pool", lambda e: e.tensor_tensor(out=msD[a][:], in0=stg[a][0][:], in1=stg[a][1][:],
                                                             op=ALU.mult),
                           reads=[t_stg[a][0], t_stg[a][1]], writes=[t_msD[a]])
                        dma(mix_s[4 + h, :, blk], msD[a][:], reads=[t_msD[a]], writes=[t_mix[4 + h][i]])

                    for h in range(4):
                        a = h % 2
                        for sm in range(2):
                            njobs = nch * 4
                            bufs = {}

                            def load_diff(kc):
                                bb = cc["ld2"] % 2
                                cc["ld2"] += 1
                                dma(kdk[bb][:], kdT_s[h, :, kc * 512:(kc + 1) * 512],
                                    reads=[t_kds[h][kc]], writes=[t_kdk[bb]])
                                dma(kdv[bb][:], vd_s[4 * kc:4 * kc + 4, :, 128 * h:128 * h + 128].rearrange("t p c -> p t c"),
                                    reads=t_vds[4 * kc:4 * kc + 4], writes=[t_kdv[bb]])
                                bufs[kc] = bb

                            def emitPV2(j):
                                kc, kt = divmod(j, 4)
                                bb = bufs[kc]
                                pbuf = j % 4
                                first = (j == 0)
                                last = (j == njobs - 1)
                                op("pe", lambda e: e.matmul(ps[0][:, :], lhsT=kdv[bb][:, kt, :], rhs=Pb[pbuf][:],
                                                            start=first, stop=last),
                                   reads=[t_kdv[bb], t_Pb[pbuf]], writes=[pst[0]])
                                op("pe", lambda e: e.matmul(ps[1][:, :], lhsT=ones_bf[:], rhs=Pb[pbuf][:],
                                                            start=first, stop=last),
                                   reads=[t_ones, t_Pb[pbuf]], writes=[pst[1]])

                            load_diff(0)
                            pb_ = 64 * sm
                            for j in range(njobs):
                                kc, kt = divmod(j, 4)
                                if kt == 2 and kc + 1 < nch:
                                    load_diff(kc + 1)
                                bb = bufs[kc]
                                sbank = 2 + cc["s"] % 2
                                cc["s"] += 1
                                r = kc - (nch - 4)
                                op("pe", lambda e: e.matmul(
                                    ps[sbank][:, :], lhsT=kdk[bb][:, kt * 128:(kt + 1) * 128],
                                    rhs=QbD[:, 2 * h + sm, :], start=True, stop=(r < 0)),
                                   reads=[t_kdk[bb], t_QbD], writes=[pst[sbank]])
                                if r >= 0:
                                    s0 = r * 512 + kt * 128
                                    for qt in range(4):
                                        op("pe", lambda e: e.matmul(
                                            ps[sbank][:, qt * 128:(qt + 1) * 128], lhsT=cmask[:, qt, s0:s0 + 128],
                                            rhs=id8[:], start=False, stop=(qt == 3)),
                                           reads=[t_cm, t_id8], writes=[pst[sbank]])
                                pbuf = j % 4
                                op("act", lambda e: e.activation(out=Pb[pbuf][:], in_=ps[sbank][:, :], func=AF.Exp,
                                                                 scale=0.125), reads=[pst[sbank]], writes=[t_Pb[pbuf]])
                                if j >= 2:
                                    emitPV2(j - 2)
                                if j == 8 and pending:
                                    fin_stage3(pending.pop(0))
                                yield "c"
                            for j in range(njobs - 2, njobs):
                                emitPV2(j)
                            op("act", lambda e: e.copy(out=stg[a][2 * sm][:], in_=ps[0][:, :]),
                               reads=[pst[0]], writes=[t_stg[a][2 * sm]])
                            op("act", lambda e: e.activation(out=stg[a][2 * sm + 1][:], in_=ps[1][:, :], func=AF.Ln),
                               reads=[pst[1]], writes=[t_stg[a][2 * sm + 1]])
                            op("act", lambda e: e.activation(out=stg[a][2 * sm + 1][:], in_=stg[a][2 * sm + 1][:],
                                                             func=AF.Exp, scale=-1.0),
                               reads=[t_stg[a][2 * sm + 1]], writes=[t_stg[a][2 * sm + 1]])
                        op("pool", lambda e: e.tensor_tensor(out=stg[a][0][:], in0=stg[a][0][:], in1=stg[a][1][:], op=ALU.mult),
                           reads=[t_stg[a][0], t_stg[a][1]], writes=[t_stg[a][0]])
                        op("pool", lambda e: e.tensor_tensor(out=stg[a][2][:], in0=stg[a][2][:], in1=stg[a][3][:], op=ALU.mult),
                           reads=[t_stg[a][2], t_stg[a][3]], writes=[t_stg[a][2]])
                        op("pool", lambda e: e.tensor_scalar(out=stg[a][2][:], in0=stg[a][2][:], scalar1=neglam[:, 0:1],
                                                             scalar2=None, op0=ALU.mult),
                           reads=[t_stg[a][2], t_lam], writes=[t_stg[a][2]])
                        op("pool", lambda e: e.tensor_tensor(out=stg[a][0][:], in0=stg[a][0][:], in1=stg[a][2][:], op=ALU.add),
                           reads=[t_stg[a][0], t_stg[a][2]], writes=[t_stg[a][0]])
                        pending.append(h)
                    yield "cfin"
                    while pending:
                        fin_stage3(pending.pop(0))
                    yield "cdone"

                def drain(g):
                    for _ in g:
                        pass

                def advance(g, until):
                    for x in g:
                        if x == until:
                            return True
                    return False

                import itertools
                slots_l = list(slots)
                nreg = len(slots_l)
                for k in range(nreg + 1):
                    parts = []
                    nB = 0
                    if k >= 1:
                        parts.append(gen_2b(slots_l[k - 1]))
                        nB += 32 * 4 * (slots_l[k - 1] + 1)
                    if k < nreg:
                        parts.append(gen_2c(slots_l[k]))
                        nB += 32 * 4 * (slots_l[k] + 1)
                    gb = itertools.chain(*parts)
                    if k < nreg:
                        ga = gen_2a(slots_l[k])
                        per = (nB + 3) // 4
                        alive = True
                        for qt in range(4):
                            advance(ga, "bisected")
                            j = 0
                            while alive and j < per:
                                x = next(gb, None)
                                if x is None:
                                    alive = False
                                    break
                                if x in ("b", "c"):
                                    j += 1
                        drain(ga)
                    drain(gb)
                kb.barrier()
                if debug == 2:
                    dma(dbg["thr"][:, :], thrf[:], reads=t_thrf)
                    kb.barrier()

        phase2([0, 1] if debug == 2 else [0, 1, 2, 3])
        if debug == 2:
            for f in range(8):
                dma(dbg["mixT"][:, f, :], mix_s[f, :, :], reads=t_mix[f])
            kb.barrier()
            return nc

        s12.close()

        with ExitStack() as s3:
            Wout = sb("Wout", [128, 8, D], BF16, s3); t_wo = toks(8)
            Wup = sb("Wup", [128, 8, DFF], BF16, s3); t_wu = toks(8)
            Wdn = sb("Wdn", [128, 32, D], BF16, s3); t_wd = toks(32)
            for t in range(8):
                dma(Wout[:, t, :], wo_b[t, :, :], reads=[t_wob[t]], writes=[t_wo[t]])
            for t in range(8):
                dma(Wup[:, t, :], wu_b[t, :, :], reads=[t_wub[t]], writes=[t_wu[t]])
            for t in range(32):
                dma(Wdn[:, t, :], wd_b[t, :, :], reads=[t_wdb[t]], writes=[t_wd[t]])
            gpost = sb("gpost_sb", [128, 2, D], F32, s3); t_gp = Tok()
            for r in range(2):
                dma(gpost[:, r, :], gpost_d[r:r + 1, :].partition_broadcast(128), writes=[t_gp])
            x1s = [sb(f"x1_{k}", [128, 2, D], F32, s3) for k in range(2)]; t_x1s = [toks(2), toks(2)]
            mixl = [sb(f"mixl{k}", [128, 8, 128], BF16, s3) for k in range(2)]; t_mixl = toks(2)
            tmpP = sb("tmpP", [128, D], F32, s3); t_tmpP = Tok()
            tmpE = sb("tmpE", [128, D], F32, s3); t_tmpE = Tok()
            h2 = sb("h2", [128, D], BF16, s3); t_h2 = Tok()
            h2Ts = [sb(f"h2T{k}", [128, 8, 256], BF16, s3) for k in range(2)]; t_h2Ts = [toks(2), toks(2)]
            rr = [sb(f"rr{i}", [128, 256], BF16, s3) for i in range(2)]; t_rr = toks(2)
            u2 = [sb(f"u2{i}", [128, 256], BF16, s3) for i in range(2)]; t_u2 = toks(2)
            ssvP = sb("ssvP", [128, 8], F32, s3); t_ssP = Tok()
            ssvE = sb("ssvE", [128, 8], F32, s3); t_ssE = Tok()
            psT = ps[6][:, :].bitcast(BF16)

            def rstd_from(ssv, t_ss, c0, c1, cres, n):
                if c1 is not None:
                    op("dve", lambda e: e.tensor_tensor(out=ssv[:, cres:cres + 1], in0=ssv[:, c0:c0 + 1],
                                                        in1=ssv[:, c1:c1 + 1], op=ALU.add), reads=[t_ss], writes=[t_ss])
                    c0 = cres
                op("dve", lambda e: e.tensor_scalar(out=ssv[:, cres:cres + 1], in0=ssv[:, c0:c0 + 1], scalar1=1.0 / n,
                                                    scalar2=EPS, op0=ALU.mult, op1=ALU.add), reads=[t_ss], writes=[t_ss])
                op("act", lambda e: e.activation(out=ssv[:, cres:cres + 1], in_=ssv[:, cres:cres + 1], func=AF.Ln),
                   reads=[t_ss], writes=[t_ss])
                op("act", lambda e: e.activation(out=ssv[:, cres:cres + 1], in_=ssv[:, cres:cres + 1], func=AF.Exp,
                                                 scale=-0.5), reads=[t_ss], writes=[t_ss])

            def prologue(gi):
                x1 = x1s[gi % 2]; t_x1 = t_x1s[gi % 2]
                h2T = h2Ts[gi % 2]; t_h2T = t_h2Ts[gi % 2]
                for tt in range(2):
                    tok0 = gi * 256 + tt * 128
                    si = tok0 // 512
                    dma(x1[:, tt, :], xo_d[tok0:tok0 + 128, :], writes=[t_x1[tt]])
                    dma(mixl[tt][:], mix_s[:, :, tok0:tok0 + 128].rearrange("f p n -> p f n"),
                        reads=[t_mix[f][si] for f in range(8)], writes=[t_mixl[tt]])
                    yield
                    for half in range(2):
                        for f in range(8):
                            op("pe", lambda e: e.matmul(ps[6 + half][:, :], lhsT=mixl[tt][:, f, :],
                                                        rhs=Wout[:, f, half * 512:(half + 1) * 512],
                                                        start=(f == 0), stop=(f == 7)),
                               reads=[t_mixl[tt], t_wo[f]], writes=[pst[6 + half]])
                    yield
                    for half in range(2):
                        op("act", lambda e: e.activation(out=tmpP[:, half * 512:(half + 1) * 512], in_=ps[6 + half][:, :],
                                                         func=AF.Square, accum_out=ssvP[:, half:half + 1]),
                           reads=[pst[6 + half]], writes=[t_tmpP, t_ssP])
                    rstd_from(ssvP, t_ssP, 0, 1, 2, D)
                    for half in range(2):
                        op("dve", lambda e: e.scalar_tensor_tensor(
                            out=tmpP[:, half * 512:(half + 1) * 512], in0=ps[6 + half][:, :], scalar=ssvP[:, 2:3],
                            in1=gpost[:, 0, half * 512:(half + 1) * 512], op0=ALU.mult, op1=ALU.mult),
                           reads=[pst[6 + half], t_ssP, t_gp], writes=[t_tmpP])
                    op("pool", lambda e: e.tensor_tensor(out=x1[:, tt, :], in0=x1[:, tt, :], in1=tmpP[:], op=ALU.add),
                       reads=[t_tmpP, t_x1[tt]], writes=[t_x1[tt]])
                    yield
                    op("act", lambda e: e.activation(out=tmpP[:], in_=x1[:, tt, :], func=AF.Square,
                                                     accum_out=ssvP[:, 3:4]), reads=[t_x1[tt]], writes=[t_tmpP, t_ssP])
                    rstd_from(ssvP, t_ssP, 3, None, 4, D)
                    op("dve", lambda e: e.tensor_scalar(out=h2[:], in0=x1[:, tt, :], scalar1=ssvP[:, 4:5], scalar2=None,
                                                        op0=ALU.mult), reads=[t_x1[tt], t_ssP], writes=[t_h2])
                    yield
                    for f in range(8):
                        op("pe", lambda e: e.transpose(out=psT[:, f * 128:(f + 1) * 128], in_=h2[:, f * 128:(f + 1) * 128],
                                                       identity=ident[:]), reads=[t_h2, t_ident], writes=[pst[6]])
                    op("dve", lambda e: e.tensor_copy(out=h2T[:, :, tt * 128:(tt + 1) * 128],
                                                      in_=psT.rearrange("p (f n) -> p f n", f=8)),
                       reads=[pst[6]], writes=[t_h2T[tt]])
                    yield

            def drain3(g):
                for _ in g:
                    pass

            drain3(prologue(0))
            for gi in range(8):
                x1 = x1s[gi % 2]; t_x1 = t_x1s[gi % 2]
                h2T = h2Ts[gi % 2]; t_h2T = t_h2Ts[gi % 2]
                nxt = prologue(gi + 1) if gi + 1 < 8 else iter(())

                def emitDown(ff):
                    k = ff % 2
                    for tt in range(2):
                        for half in range(2):
                            op("pe", lambda e: e.matmul(ps[tt * 2 + half][:, :], lhsT=u2[k][:, tt * 128:(tt + 1) * 128],
                                                        rhs=Wdn[:, ff, half * 512:(half + 1) * 512],
                                                        start=(ff == 0), stop=(ff == 31)),
                               reads=[t_u2[k], t_wd[ff]], writes=[pst[tt * 2 + half]])

                for ff in range(32):
                    k = ff % 2
                    ub = 4 + k
                    for f in range(8):
                        op("pe", lambda e: e.matmul(ps[ub][:, 0:256], lhsT=Wup[:, f, ff * 128:(ff + 1) * 128],
                                                    rhs=h2T[:, f, :], start=(f == 0), stop=(f == 7)),
                           reads=[t_wu[f], t_h2T[0], t_h2T[1]], writes=[pst[ub]])
                    op("act", lambda e: e.activation(out=rr[k][:], in_=ps[ub][:, 0:256], func=AF.Relu),
                       reads=[pst[ub]], writes=[t_rr[k]])
                    op("pool", lambda e: e.tensor_tensor(out=u2[k][:], in0=rr[k][:], in1=rr[k][:], op=ALU.mult),
                       reads=[t_rr[k]], writes=[t_u2[k]])
                    if ff >= 1:
                        emitDown(ff - 1)
                    if ff >= 4 and ff % 2 == 0:
                        next(nxt, None)
                emitDown(31)
                drain3(nxt)

                for tt in range(2):
                    tok0 = gi * 256 + tt * 128
                    for half in range(2):
                        op("act", lambda e: e.activation(out=tmpE[:, half * 512:(half + 1) * 512],
                                                         in_=ps[tt * 2 + half][:, :], func=AF.Square,
                                                         accum_out=ssvE[:, 5 + half:6 + half]),
                           reads=[pst[tt * 2 + half]], writes=[t_tmpE, t_ssE])
                    rstd_from(ssvE, t_ssE, 5, 6, 7, D)
                    for half in range(2):
                        op("dve", lambda e: e.scalar_tensor_tensor(
                            out=tmpE[:, half * 512:(half + 1) * 512], in0=ps[tt * 2 + half][:, :], scalar=ssvE[:, 7:8],
                            in1=gpost[:, 1, half * 512:(half + 1) * 512], op0=ALU.mult, op1=ALU.mult),
                           reads=[pst[tt * 2 + half], t_ssE, t_gp], writes=[t_tmpE])
                    op("pool", lambda e: e.tensor_tensor(out=x1[:, tt, :], in0=x1[:, tt, :], in1=tmpE[:], op=ALU.add),
                       reads=[t_tmpE, t_x1[tt]], writes=[t_x1[tt]])
                    dma(out_d[tok0:tok0 + 128, :], x1[:, tt, :], reads=[t_x1[tt]])
            kb.barrier()
        return nc
    return nc


def host_inputs(inputs):
    x = np.asarray(inputs["x"], np.float32)
    w_in = np.asarray(inputs["w_in"], np.float32)[0]
    sp = np.cumsum([512, 512, 512, 512, 64, 8, 512, 512, 512])
    qa, ka, va, qi, ki, wi, qd, kd, vd = np.split(w_in, sp[:-1], axis=1)

    def perm(w):
        n = w.shape[1] // 64
        w4 = w.reshape(w.shape[0], n, 2, 32)
        return w4[:, :, ::-1, :].reshape(w.shape[0], n * 64)

    wk = np.concatenate([ka, kd, ki, ki, va, vd], axis=1)
    wq = np.concatenate([qa, qd, qi, wi], axis=1)
    assert wk.shape[1] == WK_COLS and wq.shape[1] == WQ_COLS
    wk = np.ascontiguousarray(wk)
    wq = np.ascontiguousarray(wq)

    inv = (1.0 / (np.float32(10000.0) ** (np.arange(0, 64, 2, dtype=np.float32) / np.float32(64)))).astype(np.float32)
    ang = (np.arange(S, dtype=np.float32)[:, None] * inv[None, :]).astype(np.float32)
    cos = np.cos(ang).astype(np.float32)
    sin = np.sin(ang).astype(np.float32)
    cosT = np.concatenate([cos, cos, cos, cos], axis=1).T
    sinT = np.concatenate([-sin, sin, -sin, sin], axis=1).T
    cosT = np.ascontiguousarray(cosT, dtype=np.float32)
    sinT = np.ascontiguousarray(sinT, dtype=np.float32)

    def g8(v):
        return np.ascontiguousarray(np.asarray(v, np.float32)[0].reshape(8, 128).T)

    gpost = np.stack([np.asarray(inputs["g_post_mix"], np.float32)[0], np.asarray(inputs["g_post_mlp"], np.float32)[0]])
    lam = np.stack([np.asarray(inputs[k], np.float32)[0] for k in ("lambda_q1", "lambda_k1", "lambda_q2", "lambda_k2")])
    ident = np.eye(128, dtype=np.float32).astype(ml_dtypes.bfloat16)
    partner = np.arange(128) ^ 32
    permm = np.zeros((128, 128), np.float32)
    permm[partner, np.arange(128)] = 1.0
    permm = permm.astype(ml_dtypes.bfloat16)
    pow2 = np.tile((2.0 ** -np.arange(N_IT + 1, dtype=np.float64)).astype(np.float32)[None, :], (128, 1))
    common = {
        "wk": wk, "wq": wq,
        "wout": np.ascontiguousarray(np.asarray(inputs["w_out"], np.float32)[0]),
        "wup": np.ascontiguousarray(np.asarray(inputs["w_up"], np.float32)[0]),
        "wdn": np.ascontiguousarray(np.asarray(inputs["w_down"], np.float32)[0]),
        "gpm": g8(inputs["g_pre_mix"]), "gpl": g8(inputs["g_pre_mlp"]),
        "gpost": np.ascontiguousarray(gpost),
        "gds": np.ascontiguousarray(np.asarray(inputs["g_diff_sub"], np.float32)[0].reshape(128, 1)),
        "lam": np.ascontiguousarray(lam),
        "cosf": cosT, "sinf": sinT, "ident": ident, "pow2": pow2, "permm": permm,
    }
    maps = []
    owns = []
    for c in range(8):
        b, j = c // 4, c % 4
        own = np.concatenate([np.arange(512) + 512 * (4 * i + j) for i in range(4)])
        owns.append((b, own))
        xb = x[b]
        ql = np.arange(512)[:, None]
        sr = np.arange(2048)[None, :]
        cm = np.where(sr <= 512 * j + ql, 0.0, NEG).astype(np.float32)
        cm = cm.reshape(4, 128, 2048).transpose(1, 0, 2)
        m = dict(common)
        m.update({
            "xT": np.ascontiguousarray(xb.T),
            "xTo": np.ascontiguousarray(xb[own].T),
            "xo": np.ascontiguousarray(xb[own]),
            "coso": np.ascontiguousarray(cosT[:, own]),
            "sino": np.ascontiguousarray(sinT[:, own]),
            "cmask": np.ascontiguousarray(cm).astype(ml_dtypes.bfloat16),
        })
        maps.append(m)
    return maps, owns


def kernel(**inputs):
    maps, owns = host_inputs(inputs)
    nc = build()
    res = run_bass_kernel_spmd(nc, maps, core_ids=list(range(8)))
    out = np.zeros((2, S, D), np.float32)
    for c in range(8):
        b, own = owns[c]
        out[b, own] = np.asarray(res.results[c]["out"], np.float32)
    return out
```

```python
import math
import numpy as np
import ml_dtypes
import concourse.bass as bass
import concourse.mybir as mybir
from concourse.bass_utils import run_bass_kernel_spmd

F32 = mybir.dt.float32
BF16 = mybir.dt.bfloat16
ALU = mybir.AluOpType
AF = mybir.ActivationFunctionType
AX = mybir.AxisListType

S = 8192
D = 1024
DFF = 4096
NOWN = 2048
EPS = 1e-6
NEG = -30000.0
N_IT = 20
IDX_SCALE = (8 ** -0.5) * (64 ** -0.5)
LAM_INIT = 0.8 - 0.6 * math.exp(-0.3 * 0)

KA, KD, KI, VA, VD = 0, 512, 1024, 1152, 1664
WK_COLS = 2176
QA, QD, QI, WI = 0, 512, 1024, 1536
WQ_COLS = 1544


class Tok:
    __slots__ = ("w", "r", "psum")

    def __init__(self, psum=False):
        self.w = None
        self.r = []
        self.psum = psum


class Eng:
    def __init__(self, name, h, sem):
        self.name = name
        self.h = h
        self.sem = sem
        self.cnt = 0
        self.seen = {}


class KB:
    def __init__(self, nc, sems, ndma=24):
        self.nc = nc
        self.e = {
            "pe": Eng("pe", nc.tensor, sems[0]),
            "act": Eng("act", nc.scalar, sems[1]),
            "dve": Eng("dve", nc.vector, sems[2]),
            "pool": Eng("pool", nc.gpsimd, sems[3]),
            "sp": Eng("sp", nc.sync, None),
        }
        self.dsems = [[s, 0] for s in sems[4:4 + ndma]]
        self.drr = 0

    def _wait(self, eng, ev):
        sem, val = ev
        k = id(sem)
        if eng.seen.get(k, 0) >= val:
            return
        eng.h.wait_ge(sem, val)
        eng.seen[k] = val

    def _deps(self, eng, reads, writes):
        for t in reads:
            if t.w is not None:
                if not (eng.name == "pe" and t.w[0] is eng.sem):
                    self._wait(eng, t.w)
            if t.psum:
                for ev in t.r:
                    if ev[0] is not eng.sem:
                        self._wait(eng, ev)
        for t in writes:
            if t.w is not None:
                if not (eng.name == "pe" and t.w[0] is eng.sem):
                    self._wait(eng, t.w)
            for ev in t.r:
                if ev[0] is eng.sem and eng.name == "pe":
                    continue
                self._wait(eng, ev)

    def op(self, en, fn, reads=(), writes=()):
        eng = self.e[en]
        self._deps(eng, reads, writes)
        inst = fn(eng.h)
        eng.cnt += 1
        inst.then_inc(eng.sem, 1)
        ev = (eng.sem, eng.cnt)
        for t in reads:
            t.r.append(ev)
        for t in writes:
            t.w = ev
            t.r = []
        return inst

    def dma(self, out, in_, reads=(), writes=(), q="sp"):
        eng = self.e[q]
        ds = self.dsems[self.drr]
        self.drr = (self.drr + 1) % len(self.dsems)
        if ds[1] > 0:
            self._wait(eng, (ds[0], ds[1]))
        self._deps(eng, reads, writes)
        inst = eng.h.dma_start(out=out, in_=in_)
        ds[1] += 16
        inst.then_inc(ds[0], 16)
        ev = (ds[0], ds[1])
        for t in reads:
            t.r.append(ev)
        for t in writes:
            t.w = ev
            t.r = []
        return ev

    def barrier(self):
        names = ["pe", "act", "dve", "pool", "sp"]
        for a in names:
            ea = self.e[a]
            for b in names:
                eb = self.e[b]
                if a == b or eb.sem is None or eb.cnt == 0:
                    continue
                self._wait(ea, (eb.sem, eb.cnt))
            for ds in self.dsems:
                if ds[1] > 0:
                    self._wait(ea, (ds[0], ds[1]))


def toks(n):
    return [Tok() for _ in range(n)]


def build(debug=0):
    nc = bass.Bass("TRN2", target_bir_lowering=False)

    def din(name, shape, dt=F32):
        return nc.dram_tensor(name, list(shape), dt, kind="ExternalInput").ap()

    xT_d = din("xT", [D, S])
    xTo_d = din("xTo", [D, NOWN])
    xo_d = din("xo", [NOWN, D])
    wk_d = din("wk", [D, WK_COLS])
    wq_d = din("wq", [D, WQ_COLS])
    wout_d = din("wout", [D, D])
    wup_d = din("wup", [D, DFF])
    wdn_d = din("wdn", [DFF, D])
    gpm_d = din("gpm", [128, 8])
    gpl_d = din("gpl", [128, 8])
    gpost_d = din("gpost", [2, D])
    gds_d = din("gds", [128, 1])
    lam_d = din("lam", [4, 64])
    cos_d = din("cosf", [128, S])
    sin_d = din("sinf", [128, S])
    coso_d = din("coso", [128, NOWN])
    sino_d = din("sino", [128, NOWN])
    cm_d = din("cmask", [128, 4, 2048], BF16)
    id_d = din("ident", [128, 128], BF16)
    pm_d = din("permm", [128, 128], BF16)
    p2_d = din("pow2", [128, N_IT + 1])

    out_d = nc.dram_tensor("out", [NOWN, D], F32, kind="ExternalOutput").ap()
    kaT_s = nc.dram_tensor("kaT_s", [4, 128, S], BF16, kind="Internal").ap()
    kdT_s = nc.dram_tensor("kdT_s", [4, 128, S], BF16, kind="Internal").ap()
    va_s = nc.dram_tensor("va_s", [64, 128, 512], BF16, kind="Internal").ap()
    vd_s = nc.dram_tensor("vd_s", [64, 128, 512], BF16, kind="Internal").ap()
    q_s = nc.dram_tensor("q_s", [3, 4, 128, NOWN], BF16, kind="Internal").ap()
    mix_s = nc.dram_tensor("mix_s", [8, 128, NOWN], BF16, kind="Internal").ap()
    dbg = {}
    if debug:
        dbg["kiT"] = nc.dram_tensor("dbg_kiT", [128, S], BF16, kind="ExternalOutput").ap()
        dbg["kaT"] = nc.dram_tensor("dbg_kaT", [4, 128, S], BF16, kind="ExternalOutput").ap()
        dbg["va"] = nc.dram_tensor("dbg_va", [64, 128, 512], BF16, kind="ExternalOutput").ap()
        dbg["qaT"] = nc.dram_tensor("dbg_qaT", [128, 4, NOWN], BF16, kind="ExternalOutput").ap()
        dbg["wtok"] = nc.dram_tensor("dbg_wtok", [128, 16, 8], F32, kind="ExternalOutput").ap()
        dbg["mixT"] = nc.dram_tensor("dbg_mixT", [128, 8, NOWN], BF16, kind="ExternalOutput").ap()
        dbg["thr"] = nc.dram_tensor("dbg_thr", [128, 16], F32, kind="ExternalOutput").ap()

    from contextlib import ExitStack
    es = ExitStack()
    with es:
        sems = [es.enter_context(nc.semaphore(f"s{i}")) for i in range(4 + 24)]
        kb = KB(nc, sems)
        op, dma = kb.op, kb.dma

        def sb(name, shape, dt, stack=es):
            return stack.enter_context(nc.sbuf_tensor(name, list(shape), dt))

        ps = [es.enter_context(nc.psum_tensor(f"ps{i}", [128, 512], F32)) for i in range(8)]
        pst = [Tok(psum=True) for _ in range(8)]

        ident = sb("ident_sb", [128, 128], BF16); t_ident = Tok()
        ones_bf = sb("ones_bf", [128, 128], BF16); t_ones = Tok()
        neglam = sb("neglam", [128, 1], F32); t_lam = Tok()
        gds = sb("gds_sb", [128, 1], F32); t_gds = Tok()
        pow2 = sb("pow2_sb", [128, N_IT + 1], F32); t_p2 = Tok()
        epst = sb("epst", [128, 1], F32); t_eps = Tok()
        t_mix = [[Tok() for _ in range(4)] for _ in range(8)]

        dma(ident[:], id_d[:, :], writes=[t_ident])
        permt = sb("permt", [128, 128], BF16); t_perm = Tok()
        dma(permt[:], pm_d[:, :], writes=[t_perm])
        dma(pow2[:], p2_d[:, :], writes=[t_p2])
        op("pool", lambda e: e.memset(ones_bf[:], 1.0), writes=[t_ones])
        op("pool", lambda e: e.memset(epst[:], EPS), writes=[t_eps])

        with ExitStack() as s0:
            lamt = sb("lamt", [128, 4, 64], F32, s0); t_lamt = Tok()
            lprod = sb("lprod", [128, 2, 64], F32, s0); t_lp = Tok()
            lsum = sb("lsum", [128, 2], F32, s0); t_ls = Tok()
            lexp = sb("lexp", [128, 2], F32, s0); t_le = Tok()
            for r in range(4):
                dma(lamt[:, r, :], lam_d[r:r + 1, :].partition_broadcast(128), writes=[t_lamt])
            op("dve", lambda e: e.tensor_tensor(out=lprod[:, 0, :], in0=lamt[:, 0, :], in1=lamt[:, 1, :], op=ALU.mult),
               reads=[t_lamt], writes=[t_lp])
            op("dve", lambda e: e.tensor_tensor(out=lprod[:, 1, :], in0=lamt[:, 2, :], in1=lamt[:, 3, :], op=ALU.mult),
               reads=[t_lamt], writes=[t_lp])
            op("dve", lambda e: e.tensor_reduce(out=lsum[:, :], in_=lprod[:, :, :], axis=AX.X, op=ALU.add),
               reads=[t_lp], writes=[t_ls])
            op("act", lambda e: e.activation(out=lexp[:, :], in_=lsum[:, :], func=AF.Exp), reads=[t_ls], writes=[t_le])
            op("dve", lambda e: e.tensor_tensor(out=neglam[:, :], in0=lexp[:, 1:2], in1=lexp[:, 0:1], op=ALU.subtract),
               reads=[t_le], writes=[t_lam])
            op("dve", lambda e: e.tensor_scalar(out=neglam[:, :], in0=neglam[:, :], scalar1=-LAM_INIT, scalar2=None, op0=ALU.add),
               reads=[t_lam], writes=[t_lam])
            dma(gds[:], gds_d[:, :], writes=[t_gds])
            op("dve", lambda e: e.tensor_scalar(out=gds[:, :], in0=gds[:, :], scalar1=1.0 - LAM_INIT, scalar2=None, op0=ALU.mult),
               reads=[t_gds], writes=[t_gds])
            kb.barrier()

        s12 = es.enter_context(ExitStack())
        kiT = sb("kiT", [128, S], BF16, s12); t_ki = toks(16)
        t_qs = [[[Tok() for _ in range(4)] for _ in range(4)] for _ in range(3)]
        wtok = sb("wtok", [128, 16, 8], F32, s12); t_wtok = toks(16)
        t_kas = [[Tok() for _ in range(16)] for _ in range(4)]
        t_kds = [[Tok() for _ in range(16)] for _ in range(4)]
        t_vas = toks(64)
        t_vds = toks(64)

        def phase1():
            with ExitStack() as s1:
                sfx = "_p1"
                gpm = sb("gpm_sb" + sfx, [128, 8], F32, s1); t_gpm = Tok()
                dma(gpm[:], gpm_d[:, :], writes=[t_gpm])
                Wk = sb("Wk", [128, 8, WK_COLS], BF16, s1); twk = toks(8)
                Wq = sb("Wq", [128, 8, WQ_COLS], BF16, s1); twq = toks(8)
                NW1 = 4
                wst = [sb(f"wst{i}" + sfx, [128, 1024], F32, s1) for i in range(NW1)]; t_wst = toks(NW1)
                n = 0
                for (w_d, Wb, twb, ncol) in ((wk_d, Wk, twk, WK_COLS), (wq_d, Wq, twq, WQ_COLS)):
                    w_v = w_d.rearrange("(t p) c -> t p c", p=128)
                    for t in range(8):
                        for c0 in range(0, ncol, 1024):
                            c1 = min(ncol, c0 + 1024)
                            bq = n % NW1
                            dma(wst[bq][:, :c1 - c0], w_v[t, :, c0:c1], writes=[t_wst[bq]])
                            en = ("dve", "pool", "act")[n % 3]
                            if en == "act":
                                op(en, lambda e: e.activation(out=Wb[:, t, c0:c1], in_=wst[bq][:, :c1 - c0], func=AF.Copy,
                                                              scale=gpm[:, t:t + 1]),
                                   reads=[t_wst[bq], t_gpm], writes=[twb[t]])
                            else:
                                op(en, lambda e: e.tensor_scalar(
                                    out=Wb[:, t, c0:c1], in0=wst[bq][:, :c1 - c0], scalar1=gpm[:, t:t + 1], scalar2=None,
                                    op0=ALU.mult), reads=[t_wst[bq], t_gpm], writes=[twb[t]])
                            n += 1

                xc = [sb(f"xc{i}" + sfx, [128, 8, 512], F32, s1) for i in range(2)]; t_xc = toks(2)
                sq = sb("sq" + sfx, [128, 8, 512], BF16, s1); t_sq = Tok()
                rs1 = sb("rs1" + sfx, [128, 512], F32, s1); t_rs1 = Tok()
                rstd = sb("rstd" + sfx, [128, 512], F32, s1); t_rstd = Tok()
                hT = [sb(f"hT{i}" + sfx, [128, 8, 512], BF16, s1) for i in range(2)]; t_hT = [toks(8), toks(8)]
                cst = [sb(f"cst{i}" + sfx, [128, 512], F32, s1) for i in range(2)]; t_cst = toks(2)
                sst = [sb(f"sst{i}" + sfx, [128, 512], F32, s1) for i in range(2)]; t_sst = toks(2)
                r1 = [sb(f"r1_{i}" + sfx, [128, 512], F32, s1) for i in range(2)]; t_r1 = toks(2)
                r2 = [sb(f"r2_{i}" + sfx, [128, 512], F32, s1) for i in range(2)]; t_r2 = toks(2)
                kst = [sb(f"kst{i}" + sfx, [128, 512], BF16, s1) for i in range(4)]; t_kst = toks(4)
                vst = [sb(f"vst{i}" + sfx, [128, 512], BF16, s1) for i in range(4)]; t_vst = toks(4)
                pbf = [sb(f"pbf{i}" + sfx, [128, 512], BF16, s1) for i in range(2)]; t_pbf = toks(2)

                xT_v = xT_d.rearrange("(t p) n -> p t n", p=128)
                xTo_v = xTo_d.rearrange("(t p) n -> p t n", p=128)
                cnt = {"rope": 0, "v": 0, "pair": 0, "k": 0, "ci": 0}
                pend = []

                def norm_chunk(xsrc, csrc, ssrc, c):
                    b = cnt["ci"] % 2
                    cnt["ci"] += 1
                    dma(xc[b][:], xsrc[:, :, c * 512:(c + 1) * 512], writes=[t_xc[b]])
                    dma(cst[b][:], csrc[:, c * 512:(c + 1) * 512], writes=[t_cst[b]])
                    dma(sst[b][:], ssrc[:, c * 512:(c + 1) * 512], writes=[t_sst[b]])
                    op("act", lambda e: e.activation(out=sq[:], in_=xc[b][:], func=AF.Square),
                       reads=[t_xc[b]], writes=[t_sq])
                    for t in range(8):
                        op("pe", lambda e: e.matmul(ps[7][:, :], lhsT=ones_bf[:, :], rhs=sq[:, t, :],
                                                    start=(t == 0), stop=(t == 7)),
                           reads=[t_ones, t_sq], writes=[pst[7]])
                    op("dve", lambda e: e.tensor_scalar(out=rs1[:], in0=ps[7][:, :], scalar1=1.0 / D, scalar2=EPS,
                                                        op0=ALU.mult, op1=ALU.add), reads=[pst[7]], writes=[t_rs1])
                    op("act", lambda e: e.activation(out=rs1[:], in_=rs1[:], func=AF.Ln), reads=[t_rs1], writes=[t_rs1])
                    op("act", lambda e: e.activation(out=rstd[:], in_=rs1[:], func=AF.Exp, scale=-0.5),
                       reads=[t_rs1], writes=[t_rstd])
                    for t in range(8):
                        en = "dve" if t % 2 == 0 else "pool"
                        op(en, lambda e: e.tensor_tensor(out=hT[b][:, t, :], in0=xc[b][:, t, :], in1=rstd[:],
                                                         op=ALU.mult),
                           reads=[t_xc[b], t_rstd], writes=[t_hT[b][t]])
                    return b

                def rope_finish(item):
                    (b, pa, pb, k, dst_ap, wtoks, after) = item
                    op("pe", lambda e: e.matmul(ps[pb][:, :], lhsT=permt[:, :], rhs=pbf[k][:], start=True, stop=True),
                       reads=[t_perm, t_pbf[k]], writes=[pst[pb]])
                    op("dve", lambda e: e.tensor_tensor(out=r1[k][:], in0=ps[pa][:, :], in1=cst[b][:], op=ALU.mult),
                       reads=[pst[pa], t_cst[b]], writes=[t_r1[k]])
                    op("dve", lambda e: e.tensor_tensor(out=r2[k][:], in0=ps[pb][:, :], in1=sst[b][:], op=ALU.mult),
                       reads=[pst[pb], t_sst[b]], writes=[t_r2[k]])
                    op("pool", lambda e: e.tensor_tensor(out=dst_ap, in0=r1[k][:], in1=r2[k][:], op=ALU.add),
                       reads=[t_r1[k], t_r2[k]], writes=wtoks)
                    if after is not None:
                        after()

                def rope_tile(b, W, tw, c0, dst_ap, wtoks, after=None):
                    pp = cnt["pair"] % 2
                    cnt["pair"] += 1
                    pa, pb = 2 * pp, 2 * pp + 1
                    for t in range(8):
                        op("pe", lambda e: e.matmul(
                            ps[pa][:, :], lhsT=W[:, t, c0:c0 + 128], rhs=hT[b][:, t, :],
                            start=(t == 0), stop=(t == 7)),
                           reads=[tw[t], t_hT[b][t]], writes=[pst[pa]])
                    k = cnt["rope"] % 2
                    cnt["rope"] += 1
                    op("act", lambda e: e.copy(out=pbf[k][:], in_=ps[pa][:, :]), reads=[pst[pa]], writes=[t_pbf[k]])
                    if pend:
                        rope_finish(pend.pop(0))
                    pend.append((b, pa, pb, k, dst_ap, wtoks, after))

                def rope_flush():
                    while pend:
                        rope_finish(pend.pop(0))

                chunks = [("K", c) for c in range(16)] + [("Q", c) for c in range(4)]

                def do_norm(kind, c):
                    if kind == "K":
                        return norm_chunk(xT_v, cos_d, sin_d, c)
                    return norm_chunk(xTo_v, coso_d, sino_d, c)

                b = do_norm(*chunks[0])
                for idx, (kind, c) in enumerate(chunks):
                    if kind == "K":
                        for (base, spill, tsp) in ((KA, kaT_s, t_kas), (KD, kdT_s, t_kds)):
                            for i in range(4):
                                k = cnt["k"] % 4
                                cnt["k"] += 1

                                def after(k=k, spill=spill, i=i, c=c, tsp=tsp):
                                    dma(spill[i, :, c * 512:(c + 1) * 512], kst[k][:], reads=[t_kst[k]],
                                        writes=[tsp[i][c]])
                                rope_tile(b, Wk, twk, base + 128 * i, kst[k][:], [t_kst[k]], after)
                        rope_tile(b, Wk, twk, KI, kiT[:, c * 512:(c + 1) * 512], [t_ki[c]])
                        rope_flush()
                    else:
                        for (wh, base) in ((0, QA), (1, QD), (2, QI)):
                            for i in range(4):
                                k = cnt["k"] % 4
                                cnt["k"] += 1

                                def after(k=k, wh=wh, i=i, c=c):
                                    dma(q_s[wh, i, :, c * 512:(c + 1) * 512], kst[k][:], reads=[t_kst[k]],
                                        writes=[t_qs[wh][i][c]])
                                rope_tile(b, Wq, twq, base + 128 * i, kst[k][:], [t_kst[k]], after)
                        rope_flush()
                    b_next = do_norm(*chunks[idx + 1]) if idx + 1 < len(chunks) else None
                    if kind == "K":
                        for st in range(4):
                            for (base, spill, tsp) in ((VA, va_s, t_vas), (VD, vd_s, t_vds)):
                                bank = 4 + cnt["v"] % 2
                                k = cnt["v"] % 4
                                cnt["v"] += 1
                                for t in range(8):
                                    op("pe", lambda e: e.matmul(
                                        ps[bank][:, :], lhsT=hT[b][:, t, st * 128:(st + 1) * 128],
                                        rhs=Wk[:, t, base:base + 512], start=(t == 0), stop=(t == 7)),
                                       reads=[twk[t], t_hT[b][t]], writes=[pst[bank]])
                                op("act", lambda e: e.copy(out=vst[k][:], in_=ps[bank][:, :]),
                                   reads=[pst[bank]], writes=[t_vst[k]])
                                dma(spill[4 * c + st, :, :], vst[k][:], reads=[t_vst[k]], writes=[tsp[4 * c + st]])
                    else:
                        for st in range(4):
                            g = 4 * c + st
                            bank = 4 + cnt["v"] % 2
                            cnt["v"] += 1
                            for t in range(8):
                                op("pe", lambda e: e.matmul(
                                    ps[bank][:, 0:8], lhsT=hT[b][:, t, st * 128:(st + 1) * 128],
                                    rhs=Wq[:, t, WI:WI + 8], start=(t == 0), stop=(t == 7)),
                                   reads=[twq[t], t_hT[b][t]], writes=[pst[bank]])
                            op("dve", lambda e: e.tensor_scalar(
                                out=wtok[:, g, :], in0=ps[bank][:, 0:8], scalar1=IDX_SCALE, scalar2=None,
                                op0=ALU.mult), reads=[pst[bank]], writes=[t_wtok[g]])
                    b = b_next
                kb.barrier()

        phase1()

        if debug == 1:
            dma(dbg["kiT"][:, :], kiT[:], reads=t_ki)
            dma(dbg["wtok"][:, :, :], wtok[:], reads=t_wtok)
            for i in range(4):
                dma(dbg["qaT"][:, i, :], q_s[0, i, :, :], reads=t_qs[0][i])
                dma(dbg["kaT"][i, :, :], kaT_s[i, :, :], reads=t_kas[i])
            dma(dbg["va"][:, :, :], va_s[:, :, :], reads=t_vas)
            kb.barrier()
            return nc

        def phase2(slots):
            F8 = mybir.dt.float8e5
            with ExitStack() as s2:
                cmask = sb("cmask8", [128, 4, 2048], F8, s2); t_cm = Tok()
                id8 = sb("id8", [128, 128], F8, s2); t_id8 = Tok()
                with ExitStack() as s2t:
                    cmt = sb("cmask_tmp", [128, 4, 2048], BF16, s2t); t_cmt = Tok()
                    dma(cmt[:], cm_d[:, :, :], writes=[t_cmt])
                    op("dve", lambda e: e.tensor_copy(out=cmask[:], in_=cmt[:], saturate=False),
                       reads=[t_cmt], writes=[t_cm])
                    op("dve", lambda e: e.tensor_copy(out=id8[:], in_=ident[:], saturate=False),
                       reads=[t_ident], writes=[t_id8])
                    kb.barrier()
                QbI = sb("QbI", [128, 8, 512], BF16, s2); t_QbI = Tok()
                QbA = sb("QbA", [128, 8, 512], BF16, s2); t_QbA = Tok()
                QbD = QbA; t_QbD = t_QbA
                for (qb_, tq_) in ((QbI, t_QbI), (QbA, t_QbA)):
                    op("pool", lambda e: e.memset(qb_[:], 0.0), writes=[tq_])

                def load_qpad(qb_, tq_, wh, blk):
                    for t in range(4):
                        for half in range(2):
                            dma(qb_[64 * half:64 * half + 64, 2 * t + half, :],
                                q_s[wh, t, 64 * half:64 * half + 64, blk], reads=t_qs[wh][t], writes=[tq_])
                scores = [sb(f"score{k}", [128, S], F32, s2) for k in range(2)]; t_scs = [toks(16), toks(16)]
                mbr = sb("mbr", [128, 6, S], F8, s2)
                t_mbr = [toks(16) for _ in range(6)]
                msA = [sb(f"msA{k}", [128, 512], BF16, s2) for k in range(2)]; t_msA = toks(2)
                msD = [sb(f"msD{k}", [128, 512], BF16, s2) for k in range(2)]; t_msD = toks(2)
                Dm = [sb(f"Dm{i}", [128, 8, 128], BF16, s2) for i in range(2)]; t_Dm = toks(2)
                Rb = [sb(f"Rb{i}", [128, 512], BF16, s2) for i in range(4)]; t_Rb = toks(4)
                am = sb("am", [128, 2], F32, s2); t_am = Tok()
                steps = sb("steps", [128, N_IT + 1], F32, s2); t_steps = Tok()
                thrv = sb("thrv", [128, 2], F32, s2); t_thr = toks(2)
                thrf = sb("thrf", [128, 16], F32, s2); t_thrf = toks(16)
                cntv = sb("cntv", [128, 1], F32, s2); t_cnt = Tok()
                dv = sb("dv", [128, 1], F32, s2); t_dv = Tok()
                kck = [sb(f"kck{i}", [128, 512], BF16, s2) for i in range(2)]; t_kck = toks(2)
                kcv = [sb(f"kcv{i}", [128, 4, 128], BF16, s2) for i in range(2)]; t_kcv = toks(2)
                kva = [sb(f"kva{i}", [128, 4, 2, 128], BF16, s2) for i in range(2)]; t_kva = toks(2)
                stO = [sb(f"stO{i}", [128, 512], F32, s2) for i in range(2)]; t_stO = toks(2)
                stR = [sb(f"stR{i}", [64, 512], F32, s2) for i in range(2)]; t_stR = toks(2)
                Pb = [sb(f"Pb{i}", [128, 512], BF16, s2) for i in range(4)]; t_Pb = toks(4)
                kdk = [sb(f"kdk{i}", [128, 512], BF16, s2) for i in range(2)]; t_kdk = toks(2)
                kdv = [sb(f"kdv{i}", [128, 4, 128], BF16, s2) for i in range(2)]; t_kdv = toks(2)
                _stg = [sb(f"stg_{k}", [128, 512], F32, s2) for k in range(4)]
                stg = [_stg, _stg]
                _tstg = toks(4)
                t_stg = [_tstg, _tstg]
                _sqb = sb("sqb", [128, 512], BF16, s2); _tsqb = Tok()
                sqb = [_sqb, _sqb]; t_sqb = [_tsqb, _tsqb]
                for i in range(2):
                    op("pool", lambda e: e.memset(kva[i][:], 1.0), writes=[t_kva[i]])
                cc = {"ld": 0, "ld2": 0, "s": 0, "dm": 0}

                def build_Dm(g):
                    k = cc["dm"] % 2
                    cc["dm"] += 1
                    op("dve", lambda e: e.tensor_tensor(
                        out=Dm[k][:], in0=ident[:].unsqueeze(1).to_broadcast([128, 8, 128]),
                        in1=wtok[:, g, :].unsqueeze(2).to_broadcast([128, 8, 128]), op=ALU.mult),
                       reads=[t_ident, t_wtok[g]], writes=[t_Dm[k]])
                    return k

                def gen_2a(i):
                    nk = 2048 * (i + 1)
                    nch = 4 * (i + 1)
                    blk = slice(i * 512, (i + 1) * 512)
                    load_qpad(QbI, t_QbI, 2, blk)
                    dk = build_Dm(4 * i)
                    for qt in range(4):
                        g = 4 * i + qt
                        njobs = 8 * nch
                        score = scores[g % 2]
                        t_sc = t_scs[g % 2]
                        reg = g % 6

                        def emitD(j):
                            kc, h = divmod(j, 8)
                            sbank = 6 + kc % 2
                            op("pe", lambda e: e.matmul(ps[sbank][:, :], lhsT=Dm[dk][:, h, :], rhs=Rb[j % 4][:],
                                                        start=(h == 0), stop=(h == 7)),
                               reads=[t_Dm[dk], t_Rb[j % 4]], writes=[pst[sbank]])
                            if h == 7:
                                sl = slice(kc * 512, (kc + 1) * 512)
                                op("act", lambda e: e.copy(out=score[:, sl], in_=ps[sbank][:, :]),
                                   reads=[pst[sbank]], writes=[t_sc[kc]])

                        for j in range(njobs):
                            kc, h = divmod(j, 8)
                            pb_ = 64 * (h % 2)
                            bank = 4 + j % 2
                            op("pe", lambda e: e.matmul(
                                ps[bank][:, :], lhsT=QbI[:, h, qt * 128:(qt + 1) * 128],
                                rhs=kiT[:, kc * 512:(kc + 1) * 512], start=True, stop=True),
                               reads=[t_QbI, t_ki[kc]], writes=[pst[bank]])
                            op("act", lambda e: e.activation(out=Rb[j % 4][:], in_=ps[bank][:, :], func=AF.Relu),
                               reads=[pst[bank]], writes=[t_Rb[j % 4]])
                            if j >= 1:
                                emitD(j - 1)
                            if h == 7 and kc % 2 == 1:
                                yield "s"
                        emitD(njobs - 1)
                        yield "scored"
                        dk_next = build_Dm(g + 1) if qt < 3 else None
                        op("dve", lambda e: e.tensor_reduce(
                            out=am[:, 0:1], in_=score[:, 0:nk], axis=AX.X, op=ALU.max,
                            apply_absolute_value=True), reads=t_sc[:nch], writes=[t_am])
                        for r in range(4):
                            kc = nch - 4 + r
                            op("dve", lambda e: e.tensor_tensor(
                                out=score[:, kc * 512:(kc + 1) * 512], in0=score[:, kc * 512:(kc + 1) * 512],
                                in1=cmask[:, qt, r * 512:(r + 1) * 512], op=ALU.add),
                               reads=[t_cm, t_sc[kc]], writes=[t_sc[kc]])
                        op("dve", lambda e: e.tensor_scalar(out=am[:, 1:2], in0=am[:, 0:1], scalar1=1.001,
                                                            scalar2=1e-20, op0=ALU.mult, op1=ALU.add),
                           reads=[t_am], writes=[t_am])
                        op("dve", lambda e: e.tensor_scalar(out=steps[:], in0=pow2[:], scalar1=am[:, 1:2],
                                                            scalar2=None, op0=ALU.mult),
                           reads=[t_am, t_p2], writes=[t_steps])
                        op("dve", lambda e: e.memset(thrv[:, 0:1], 0.0), writes=[t_thr[0]])
                        for it in range(N_IT):
                            cur, nxt = it % 2, (it + 1) % 2
                            op("dve", lambda e: e.tensor_scalar(
                                out=mbr[:, reg, 0:nk], in0=score[:, 0:nk], scalar1=thrv[:, cur:cur + 1], scalar2=None,
                                op0=ALU.is_ge, op1=ALU.add, accum_out=cntv[:, 0:1], saturate=False),
                               reads=t_sc[:nch] + [t_thr[cur]], writes=t_mbr[reg][:nch] + [t_cnt])
                            op("dve", lambda e: e.tensor_scalar(out=dv[:], in0=cntv[:], scalar1=255.5, scalar2=0.5,
                                                                op0=ALU.is_ge, op1=ALU.subtract),
                               reads=[t_cnt], writes=[t_dv])
                            op("dve", lambda e: e.scalar_tensor_tensor(
                                out=thrv[:, nxt:nxt + 1], in0=dv[:], scalar=steps[:, it:it + 1], in1=thrv[:, cur:cur + 1],
                                op0=ALU.mult, op1=ALU.add), reads=[t_dv, t_steps, t_thr[cur]], writes=[t_thr[nxt]])
                        fin = N_IT % 2
                        op("dve", lambda e: e.scalar_tensor_tensor(
                            out=thrf[:, g:g + 1], in0=steps[:, N_IT:N_IT + 1], scalar=-1.0, in1=thrv[:, fin:fin + 1],
                            op0=ALU.mult, op1=ALU.add), reads=[t_steps, t_thr[fin]], writes=[t_thrf[g]])
                        op("dve", lambda e: e.tensor_scalar(
                            out=mbr[:, reg, 0:nk], in0=score[:, 0:nk], scalar1=thrf[:, g:g + 1], scalar2=NEG,
                            op0=ALU.is_lt, op1=ALU.mult, saturate=False),
                           reads=t_sc[:nch] + [t_thrf[g]], writes=t_mbr[reg][:nch])
                        dk = dk_next
                        yield "bisected"

                def gen_2b(i):
                    nch = 4 * (i + 1)
                    blk = slice(i * 512, (i + 1) * 512)
                    load_qpad(QbA, t_QbA, 0, blk)
                    for p in range(4):
                        njobs = nch * 8
                        bufs = {}

                        def load_dsa(kc):
                            bb = cc["ld"] % 2
                            cc["ld"] += 1
                            dma(kck[bb][:], kaT_s[p, :, kc * 512:(kc + 1) * 512], reads=[t_kas[p][kc]], writes=[t_kck[bb]])
                            dma(kcv[bb][:], va_s[4 * kc:4 * kc + 4, :, 128 * p:128 * p + 128].rearrange("t p c -> p t c"),
                                reads=t_vas[4 * kc:4 * kc + 4], writes=[t_kcv[bb]])
                            op("pool", lambda e: e.tensor_copy(
                                out=kva[bb][:, :, :, 0:64], in_=kcv[bb][:].rearrange("p t (h d) -> p t h d", h=2)),
                               reads=[t_kcv[bb]], writes=[t_kva[bb]])
                            bufs[kc] = bb

                        def emitPV(j):
                            kc, rem = divmod(j, 8)
                            kt, hh = divmod(rem, 2)
                            bb = bufs[kc]
                            pbuf = j % 4
                            op("pe", lambda e: e.matmul(ps[hh][:, :], lhsT=kva[bb][:, kt, hh, :], rhs=Pb[pbuf][:],
                                                        start=(kc == 0 and kt == 0), stop=(kc == nch - 1 and kt == 3)),
                               reads=[t_kva[bb], t_Pb[pbuf]], writes=[pst[hh]])

                        load_dsa(0)
                        for j in range(njobs):
                            kc, rem = divmod(j, 8)
                            kt, hh = divmod(rem, 2)
                            if rem == 2 and kc + 1 < nch:
                                load_dsa(kc + 1)
                            bb = bufs[kc]
                            sbank = 2 + cc["s"] % 2
                            cc["s"] += 1
                            op("pe", lambda e: e.matmul(
                                ps[sbank][:, :], lhsT=kck[bb][:, kt * 128:(kt + 1) * 128],
                                rhs=QbA[:, 2 * p + hh, :], start=True, stop=False),
                               reads=[t_kck[bb], t_QbA], writes=[pst[sbank]])
                            s0 = kc * 512 + kt * 128
                            for qt in range(4):
                                reg = (4 * i + qt) % 6
                                op("pe", lambda e: e.matmul(
                                    ps[sbank][:, qt * 128:(qt + 1) * 128], lhsT=mbr[:, reg, s0:s0 + 128], rhs=id8[:],
                                    start=False, stop=(qt == 3)),
                                   reads=[t_mbr[reg][kc], t_id8], writes=[pst[sbank]])
                            pbuf = j % 4
                            op("act", lambda e: e.activation(out=Pb[pbuf][:], in_=ps[sbank][:, :], func=AF.Exp,
                                                             scale=0.125), reads=[pst[sbank]], writes=[t_Pb[pbuf]])
                            if j >= 2:
                                emitPV(j - 2)
                            yield "b"
                        for j in range(njobs - 2, njobs):
                            emitPV(j)
                        k = p % 2
                        for hh in range(2):
                            op("act", lambda e: e.copy(out=stO[hh][0:64, :], in_=ps[hh][0:64, :]),
                               reads=[pst[hh]], writes=[t_stO[hh]])
                            op("act", lambda e: e.activation(out=stO[hh][64:128, :], in_=ps[hh][64:128, :], func=AF.Ln),
                               reads=[pst[hh]], writes=[t_stO[hh]])
                            op("act", lambda e: e.activation(out=stR[hh][0:64, :], in_=stO[hh][64:128, :],
                                                             func=AF.Exp, scale=-1.0),
                               reads=[t_stO[hh]], writes=[t_stR[hh]])
                            op("pool", lambda e: e.tensor_tensor(
                                out=msA[k][64 * hh:64 * hh + 64, :], in0=stO[hh][0:64, :], in1=stR[hh][0:64, :],
                                op=ALU.mult), reads=[t_stO[hh], t_stR[hh]], writes=[t_msA[k]])
                        dma(mix_s[p, :, blk], msA[k][:], reads=[t_msA[k]], writes=[t_mix[p][i]])
                    yield "bdone"

                def gen_2c(i):
                    nch = 4 * (i + 1)
                    blk = slice(i * 512, (i + 1) * 512)
                    load_qpad(QbD, t_QbD, 1, blk)
                    pending = []

                    def fin_stage3(h):
                        a = h % 2
                        sbank = 2 + cc["s"] % 2
                        cc["s"] += 1
                        op("act", lambda e: e.activation(out=sqb[a][:], in_=stg[a][0][:], func=AF.Square),
                           reads=[t_stg[a][0]], writes=[t_sqb[a]])
                        op("pe", lambda e: e.matmul(ps[sbank][:, :], lhsT=ones_bf[:], rhs=sqb[a][:], start=True, stop=True),
                           reads=[t_ones, t_sqb[a]], writes=[pst[sbank]])
                        op("act", lambda e: e.activation(out=stg[a][1][:], in_=ps[sbank][:, :], func=AF.Ln,
                                                         scale=1.0 / 128, bias=epst[:, 0:1]),
                           reads=[pst[sbank], t_eps], writes=[t_stg[a][1]])
                        op("act", lambda e: e.activation(out=stg[a][1][:], in_=stg[a][1][:], func=AF.Exp, scale=-0.5),
                           reads=[t_stg[a][1]], writes=[t_stg[a][1]])
                        op("pool", lambda e: e.tensor_scalar(out=stg[a][0][:], in0=stg[a][0][:], scalar1=gds[:, 0:1],
                                                             scalar2=None, op0=ALU.mult),
                           reads=[t_stg[a][0], t_gds], writes=[t_stg[a][0]])
                        op("pool", lambda e: e.tensor_tensor(out=msD[a][:], in0=stg[a][0][:], in1=stg[a][1][:],
                                                             op=ALU.mult),
                           reads=[t_stg[a][0], t_stg[a][1]], writes=[t_msD[a]])
                        dma(mix_s[4 + h, :, blk], msD[a][:], reads=[t_msD[a]], writes=[t_mix[4 + h][i]])

                    for h in range(4):
                        a = h % 2
                        for sm in range(2):
                            njobs = nch * 4
                            bufs = {}

                            def load_diff(kc):
                                bb = cc["ld2"] % 2
                                cc["ld2"] += 1
                                dma(kdk[bb][:], kdT_s[h, :, kc * 512:(kc + 1) * 512],
                                    reads=[t_kds[h][kc]], writes=[t_kdk[bb]])
                                dma(kdv[bb][:], vd_s[4 * kc:4 * kc + 4, :, 128 * h:128 * h + 128].rearrange("t p c -> p t c"),
                                    reads=t_vds[4 * kc:4 * kc + 4], writes=[t_kdv[bb]])
                                bufs[kc] = bb

                            def emitPV2(j):
                                kc, kt = divmod(j, 4)
                                bb = bufs[kc]
                                pbuf = j % 4
                                first = (j == 0)
                                last = (j == njobs - 1)
                                op("pe", lambda e: e.matmul(ps[0][:, :], lhsT=kdv[bb][:, kt, :], rhs=Pb[pbuf][:],
                                                            start=first, stop=last),
                                   reads=[t_kdv[bb], t_Pb[pbuf]], writes=[pst[0]])
                                op("pe", lambda e: e.matmul(ps[1][:, :], lhsT=ones_bf[:], rhs=Pb[pbuf][:],
                                                            start=first, stop=last),
                                   reads=[t_ones, t_Pb[pbuf]], writes=[pst[1]])

                            load_diff(0)
                            pb_ = 64 * sm
                            for j in range(njobs):
                                kc, kt = divmod(j, 4)
                                if kt == 2 and kc + 1 < nch:
                                    load_diff(kc + 1)
                                bb = bufs[kc]
                                sbank = 2 + cc["s"] % 2
                                cc["s"] += 1
                                r = kc - (nch - 4)
                                op("pe", lambda e: e.matmul(
                                    ps[sbank][:, :], lhsT=kdk[bb][:, kt * 128:(kt + 1) * 128],
                                    rhs=QbD[:, 2 * h + sm, :], start=True, stop=(r < 0)),
                                   reads=[t_kdk[bb], t_QbD], writes=[pst[sbank]])
                                if r >= 0:
                                    s0 = r * 512 + kt * 128
                                    for qt in range(4):
                                        op("pe", lambda e: e.matmul(
                                            ps[sbank][:, qt * 128:(qt + 1) * 128], lhsT=cmask[:, qt, s0:s0 + 128],
                                            rhs=id8[:], start=False, stop=(qt == 3)),
                                           reads=[t_cm, t_id8], writes=[pst[sbank]])
                                pbuf = j % 4
                                op("act", lambda e: e.activation(out=Pb[pbuf][:], in_=ps[sbank][:, :], func=AF.Exp,
                                                                 scale=0.125), reads=[pst[sbank]], writes=[t_Pb[pbuf]])
                                if j >= 2:
                                    emitPV2(j - 2)
                                if j == 8 and pending:
                                    fin_stage3(pending.pop(0))
                                yield "c"
                            for j in range(njobs - 2, njobs):
                                emitPV2(j)
                            op("act", lambda e: e.copy(out=stg[a][2 * sm][:], in_=ps[0][:, :]),
                               reads=[pst[0]], writes=[t_stg[a][2 * sm]])
                            op("act", lambda e: e.activation(out=stg[a][2 * sm + 1][:], in_=ps[1][:, :], func=AF.Ln),
                               reads=[pst[1]], writes=[t_stg[a][2 * sm + 1]])
                            op("act", lambda e: e.activation(out=stg[a][2 * sm + 1][:], in_=stg[a][2 * sm + 1][:],
                                                             func=AF.Exp, scale=-1.0),
                               reads=[t_stg[a][2 * sm + 1]], writes=[t_stg[a][2 * sm + 1]])
                        op("pool", lambda e: e.tensor_tensor(out=stg[a][0][:], in0=stg[a][0][:], in1=stg[a][1][:], op=ALU.mult),
                           reads=[t_stg[a][0], t_stg[a][1]], writes=[t_stg[a][0]])
                        op("pool", lambda e: e.tensor_tensor(out=stg[a][2][:], in0=stg[a][2][:], in1=stg[a][3][:], op=ALU.mult),
                           reads=[t_stg[a][2], t_stg[a][3]], writes=[t_stg[a][2]])
                        op("pool", lambda e: e.tensor_scalar(out=stg[a][2][:], in0=stg[a][2][:], scalar1=neglam[:, 0:1],
                                                             scalar2=None, op0=ALU.mult),
                           reads=[t_stg[a][2], t_lam], writes=[t_stg[a][2]])
                        op("pool", lambda e: e.tensor_tensor(out=stg[a][0][:], in0=stg[a][0][:], in1=stg[a][2][:], op=ALU.add),
                           reads=[t_stg[a][0], t_stg[a][2]], writes=[t_stg[a][0]])
                        pending.append(h)
                    yield "cfin"
                    while pending:
                        fin_stage3(pending.pop(0))
                    yield "cdone"

                def drain(g):
                    for _ in g:
                        pass

                def advance(g, until):
                    for x in g:
                        if x == until:
                            return True
                    return False

                import itertools
                slots_l = list(slots)
                nreg = len(slots_l)
                for k in range(nreg + 1):
                    parts = []
                    nB = 0
                    if k >= 1:
                        parts.append(gen_2b(slots_l[k - 1]))
                        nB += 32 * 4 * (slots_l[k - 1] + 1)
                    if k < nreg:
                        parts.append(gen_2c(slots_l[k]))
                        nB += 32 * 4 * (slots_l[k] + 1)
                    gb = itertools.chain(*parts)
                    if k < nreg:
                        ga = gen_2a(slots_l[k])
                        per = (nB + 3) // 4
                        alive = True
                        for qt in range(4):
                            advance(ga, "bisected")
                            j = 0
                            while alive and j < per:
                                x = next(gb, None)
                                if x is None:
                                    alive = False
                                    break
                                if x in ("b", "c"):
                                    j += 1
                        drain(ga)
                    drain(gb)
                kb.barrier()
                if debug == 2:
                    dma(dbg["thr"][:, :], thrf[:], reads=t_thrf)
                    kb.barrier()

        phase2([0, 1] if debug == 2 else [0, 1, 2, 3])
        if debug == 2:
            for f in range(8):
                dma(dbg["mixT"][:, f, :], mix_s[f, :, :], reads=t_mix[f])
            kb.barrier()
            return nc

        s12.close()

        with ExitStack() as s3:
            Wout = sb("Wout", [128, 8, D], BF16, s3); t_wo = toks(8)
            Wup = sb("Wup", [128, 8, DFF], BF16, s3); t_wu = toks(8)
            Wdn = sb("Wdn", [128, 32, D], BF16, s3); t_wd = toks(32)
            gpl = sb("gpl_sb", [128, 8], F32, s3); t_gpl = Tok()
            dma(gpl[:], gpl_d[:, :], writes=[t_gpl])
            with ExitStack() as s3w:
                NW3 = 8
                wst = [sb(f"wst3_{i}", [128, 1024], F32, s3w) for i in range(NW3)]; t_wst = toks(NW3)
                n = 0
                wo_v = wout_d.rearrange("(t p) c -> t p c", p=128)
                wu_v = wup_d.rearrange("(t p) c -> t p c", p=128)
                wd_v = wdn_d.rearrange("(t p) c -> t p c", p=128)
                jobs = [(wo_v[t, :, :], Wout[:, t, :], t_wo[t], None) for t in range(8)]
                jobs += [(wu_v[t, :, c0:c0 + 1024], Wup[:, t, c0:c0 + 1024], t_wu[t], t) for t in range(8)
                         for c0 in range(0, DFF, 1024)]
                jobs += [(wd_v[t, :, :], Wdn[:, t, :], t_wd[t], None) for t in range(32)]
                for (src, dst, tk, gt) in jobs:
                    bq = n % NW3
                    dma(wst[bq][:], src, writes=[t_wst[bq]])
                    en = ("dve", "pool", "act")[n % 3]
                    if gt is not None:
                        if en == "act":
                            op(en, lambda e: e.activation(out=dst, in_=wst[bq][:], func=AF.Copy, scale=gpl[:, gt:gt + 1]),
                               reads=[t_wst[bq], t_gpl], writes=[tk])
                        else:
                            op(en, lambda e: e.tensor_scalar(out=dst, in0=wst[bq][:], scalar1=gpl[:, gt:gt + 1],
                                                             scalar2=None, op0=ALU.mult),
                               reads=[t_wst[bq], t_gpl], writes=[tk])
                    else:
                        if en == "act":
                            op(en, lambda e: e.copy(out=dst, in_=wst[bq][:]), reads=[t_wst[bq]], writes=[tk])
                        else:
                            op(en, lambda e: e.tensor_copy(out=dst, in_=wst[bq][:]), reads=[t_wst[bq]], writes=[tk])
                    n += 1
                kb.barrier()
            gpost = sb("gpost_sb", [128, 2, D], F32, s3); t_gp = Tok()
            for r in range(2):
                dma(gpost[:, r, :], gpost_d[r:r + 1, :].partition_broadcast(128), writes=[t_gp])
            x1s = [sb(f"x1_{k}", [128, 2, D], F32, s3) for k in range(2)]; t_x1s = [toks(2), toks(2)]
            mixl = [sb(f"mixl{k}", [128, 8, 128], BF16, s3) for k in range(2)]; t_mixl = toks(2)
            tmpP = sb("tmpP", [128, D], F32, s3); t_tmpP = Tok()
            tmpE = sb("tmpE", [128, D], F32, s3); t_tmpE = Tok()
            h2 = sb("h2", [128, D], BF16, s3); t_h2 = Tok()
            h2Ts = [sb(f"h2T{k}", [128, 8, 256], BF16, s3) for k in range(2)]; t_h2Ts = [toks(2), toks(2)]
            rr = [sb(f"rr{i}", [128, 256], BF16, s3) for i in range(2)]; t_rr = toks(2)
            u2 = [sb(f"u2{i}", [128, 256], BF16, s3) for i in range(2)]; t_u2 = toks(2)
            ssvP = sb("ssvP", [128, 8], F32, s3); t_ssP = Tok()
            ssvE = sb("ssvE", [128, 8], F32, s3); t_ssE = Tok()
            psT = ps[6][:, :].bitcast(BF16)

            def rstd_from(ssv, t_ss, c0, c1, cres, n):
                if c1 is not None:
                    op("dve", lambda e: e.tensor_tensor(out=ssv[:, cres:cres + 1], in0=ssv[:, c0:c0 + 1],
                                                        in1=ssv[:, c1:c1 + 1], op=ALU.add), reads=[t_ss], writes=[t_ss])
                    c0 = cres
                op("dve", lambda e: e.tensor_scalar(out=ssv[:, cres:cres + 1], in0=ssv[:, c0:c0 + 1], scalar1=1.0 / n,
                                                    scalar2=EPS, op0=ALU.mult, op1=ALU.add), reads=[t_ss], writes=[t_ss])
                op("act", lambda e: e.activation(out=ssv[:, cres:cres + 1], in_=ssv[:, cres:cres + 1], func=AF.Ln),
                   reads=[t_ss], writes=[t_ss])
                op("act", lambda e: e.activation(out=ssv[:, cres:cres + 1], in_=ssv[:, cres:cres + 1], func=AF.Exp,
                                                 scale=-0.5), reads=[t_ss], writes=[t_ss])

            def prologue(gi):
                x1 = x1s[gi % 2]; t_x1 = t_x1s[gi % 2]
                h2T = h2Ts[gi % 2]; t_h2T = t_h2Ts[gi % 2]
                for tt in range(2):
                    tok0 = gi * 256 + tt * 128
                    si = tok0 // 512
                    dma(x1[:, tt, :], xo_d[tok0:tok0 + 128, :], writes=[t_x1[tt]])
                    dma(mixl[tt][:], mix_s[:, :, tok0:tok0 + 128].rearrange("f p n -> p f n"),
                        reads=[t_mix[f][si] for f in range(8)], writes=[t_mixl[tt]])
                    yield
                    for half in range(2):
                        for f in range(8):
                            op("pe", lambda e: e.matmul(ps[6 + half][:, :], lhsT=mixl[tt][:, f, :],
                                                        rhs=Wout[:, f, half * 512:(half + 1) * 512],
                                                        start=(f == 0), stop=(f == 7)),
                               reads=[t_mixl[tt], t_wo[f]], writes=[pst[6 + half]])
                    yield
                    for half in range(2):
                        op("act", lambda e: e.activation(out=tmpP[:, half * 512:(half + 1) * 512], in_=ps[6 + half][:, :],
                                                         func=AF.Square, accum_out=ssvP[:, half:half + 1]),
                           reads=[pst[6 + half]], writes=[t_tmpP, t_ssP])
                    rstd_from(ssvP, t_ssP, 0, 1, 2, D)
                    for half in range(2):
                        op("dve", lambda e: e.scalar_tensor_tensor(
                            out=tmpP[:, half * 512:(half + 1) * 512], in0=ps[6 + half][:, :], scalar=ssvP[:, 2:3],
                            in1=gpost[:, 0, half * 512:(half + 1) * 512], op0=ALU.mult, op1=ALU.mult),
                           reads=[pst[6 + half], t_ssP, t_gp], writes=[t_tmpP])
                    op("pool", lambda e: e.tensor_tensor(out=x1[:, tt, :], in0=x1[:, tt, :], in1=tmpP[:], op=ALU.add),
                       reads=[t_tmpP, t_x1[tt]], writes=[t_x1[tt]])
                    yield
                    op("act", lambda e: e.activation(out=tmpP[:], in_=x1[:, tt, :], func=AF.Square,
                                                     accum_out=ssvP[:, 3:4]), reads=[t_x1[tt]], writes=[t_tmpP, t_ssP])
                    rstd_from(ssvP, t_ssP, 3, None, 4, D)
                    op("dve", lambda e: e.tensor_scalar(out=h2[:], in0=x1[:, tt, :], scalar1=ssvP[:, 4:5], scalar2=None,
                                                        op0=ALU.mult), reads=[t_x1[tt], t_ssP], writes=[t_h2])
                    yield
                    for f in range(8):
                        op("pe", lambda e: e.transpose(out=psT[:, f * 128:(f + 1) * 128], in_=h2[:, f * 128:(f + 1) * 128],
                                                       identity=ident[:]), reads=[t_h2, t_ident], writes=[pst[6]])
                    op("dve", lambda e: e.tensor_copy(out=h2T[:, :, tt * 128:(tt + 1) * 128],
                                                      in_=psT.rearrange("p (f n) -> p f n", f=8)),
                       reads=[pst[6]], writes=[t_h2T[tt]])
                    yield

            def drain3(g):
                for _ in g:
                    pass

            drain3(prologue(0))
            for gi in range(8):
                x1 = x1s[gi % 2]; t_x1 = t_x1s[gi % 2]
                h2T = h2Ts[gi % 2]; t_h2T = t_h2Ts[gi % 2]
                nxt = prologue(gi + 1) if gi + 1 < 8 else iter(())

                def emitDown(ff):
                    k = ff % 2
                    for tt in range(2):
                        for half in range(2):
                            op("pe", lambda e: e.matmul(ps[tt * 2 + half][:, :], lhsT=u2[k][:, tt * 128:(tt + 1) * 128],
                                                        rhs=Wdn[:, ff, half * 512:(half + 1) * 512],
                                                        start=(ff == 0), stop=(ff == 31)),
                               reads=[t_u2[k], t_wd[ff]], writes=[pst[tt * 2 + half]])

                for ff in range(32):
                    k = ff % 2
                    ub = 4 + k
                    for f in range(8):
                        op("pe", lambda e: e.matmul(ps[ub][:, 0:256], lhsT=Wup[:, f, ff * 128:(ff + 1) * 128],
                                                    rhs=h2T[:, f, :], start=(f == 0), stop=(f == 7)),
                           reads=[t_wu[f], t_h2T[0], t_h2T[1]], writes=[pst[ub]])
                    op("act", lambda e: e.activation(out=rr[k][:], in_=ps[ub][:, 0:256], func=AF.Relu),
                       reads=[pst[ub]], writes=[t_rr[k]])
                    op("pool", lambda e: e.tensor_tensor(out=u2[k][:], in0=rr[k][:], in1=rr[k][:], op=ALU.mult),
                       reads=[t_rr[k]], writes=[t_u2[k]])
                    if ff >= 1:
                        emitDown(ff - 1)
                    if ff >= 4 and ff % 2 == 0:
                        next(nxt, None)
                emitDown(31)
                drain3(nxt)

                for tt in range(2):
                    tok0 = gi * 256 + tt * 128
                    for half in range(2):
                        op("act", lambda e: e.activation(out=tmpE[:, half * 512:(half + 1) * 512],
                                                         in_=ps[tt * 2 + half][:, :], func=AF.Square,
                                                         accum_out=ssvE[:, 5 + half:6 + half]),
                           reads=[pst[tt * 2 + half]], writes=[t_tmpE, t_ssE])
                    rstd_from(ssvE, t_ssE, 5, 6, 7, D)
                    for half in range(2):
                        op("dve", lambda e: e.scalar_tensor_tensor(
                            out=tmpE[:, half * 512:(half + 1) * 512], in0=ps[tt * 2 + half][:, :], scalar=ssvE[:, 7:8],
                            in1=gpost[:, 1, half * 512:(half + 1) * 512], op0=ALU.mult, op1=ALU.mult),
                           reads=[pst[tt * 2 + half], t_ssE, t_gp], writes=[t_tmpE])
                    op("pool", lambda e: e.tensor_tensor(out=x1[:, tt, :], in0=x1[:, tt, :], in1=tmpE[:], op=ALU.add),
                       reads=[t_tmpE, t_x1[tt]], writes=[t_x1[tt]])
                    dma(out_d[tok0:tok0 + 128, :], x1[:, tt, :], reads=[t_x1[tt]])
            kb.barrier()
        return nc
    return nc


def host_inputs(inputs):
    x = np.asarray(inputs["x"], np.float32)
    w_in = np.asarray(inputs["w_in"], np.float32)[0]
    sp = np.cumsum([512, 512, 512, 512, 64, 8, 512, 512, 512])
    qa, ka, va, qi, ki, wi, qd, kd, vd = np.split(w_in, sp[:-1], axis=1)

    def perm(w):
        n = w.shape[1] // 64
        w4 = w.reshape(w.shape[0], n, 2, 32)
        return w4[:, :, ::-1, :].reshape(w.shape[0], n * 64)

    wk = np.concatenate([ka, kd, ki, ki, va, vd], axis=1)
    wq = np.concatenate([qa, qd, qi, wi], axis=1)
    assert wk.shape[1] == WK_COLS and wq.shape[1] == WQ_COLS
    wk = np.ascontiguousarray(wk)
    wq = np.ascontiguousarray(wq)

    inv = (1.0 / (np.float32(10000.0) ** (np.arange(0, 64, 2, dtype=np.float32) / np.float32(64)))).astype(np.float32)
    ang = (np.arange(S, dtype=np.float32)[:, None] * inv[None, :]).astype(np.float32)
    cos = np.cos(ang).astype(np.float32)
    sin = np.sin(ang).astype(np.float32)
    cosT = np.concatenate([cos, cos, cos, cos], axis=1).T
    sinT = np.concatenate([-sin, sin, -sin, sin], axis=1).T
    cosT = np.ascontiguousarray(cosT, dtype=np.float32)
    sinT = np.ascontiguousarray(sinT, dtype=np.float32)

    def g8(v):
        return np.ascontiguousarray(np.asarray(v, np.float32)[0].reshape(8, 128).T)

    gpost = np.stack([np.asarray(inputs["g_post_mix"], np.float32)[0], np.asarray(inputs["g_post_mlp"], np.float32)[0]])
    lam = np.stack([np.asarray(inputs[k], np.float32)[0] for k in ("lambda_q1", "lambda_k1", "lambda_q2", "lambda_k2")])
    ident = np.eye(128, dtype=np.float32).astype(ml_dtypes.bfloat16)
    partner = np.arange(128) ^ 32
    permm = np.zeros((128, 128), np.float32)
    permm[partner, np.arange(128)] = 1.0
    permm = permm.astype(ml_dtypes.bfloat16)
    pow2 = np.tile((2.0 ** -np.arange(N_IT + 1, dtype=np.float64)).astype(np.float32)[None, :], (128, 1))
    common = {
        "wk": wk, "wq": wq,
        "wout": np.ascontiguousarray(np.asarray(inputs["w_out"], np.float32)[0]),
        "wup": np.ascontiguousarray(np.asarray(inputs["w_up"], np.float32)[0]),
        "wdn": np.ascontiguousarray(np.asarray(inputs["w_down"], np.float32)[0]),
        "gpm": g8(inputs["g_pre_mix"]), "gpl": g8(inputs["g_pre_mlp"]),
        "gpost": np.ascontiguousarray(gpost),
        "gds": np.ascontiguousarray(np.asarray(inputs["g_diff_sub"], np.float32)[0].reshape(128, 1)),
        "lam": np.ascontiguousarray(lam),
        "cosf": cosT, "sinf": sinT, "ident": ident, "pow2": pow2, "permm": permm,
    }
    maps = []
    owns = []
    for c in range(8):
        b, j = c // 4, c % 4
        own = np.concatenate([np.arange(512) + 512 * (4 * i + j) for i in range(4)])
        owns.append((b, own))
        xb = x[b]
        ql = np.arange(512)[:, None]
        sr = np.arange(2048)[None, :]
        cm = np.where(sr <= 512 * j + ql, 0.0, NEG).astype(np.float32)
        cm = cm.reshape(4, 128, 2048).transpose(1, 0, 2)
        m = dict(common)
        m.update({
            "xT": np.ascontiguousarray(xb.T),
            "xTo": np.ascontiguousarray(xb[own].T),
            "xo": np.ascontiguousarray(xb[own]),
            "coso": np.ascontiguousarray(cosT[:, own]),
            "sino": np.ascontiguousarray(sinT[:, own]),
            "cmask": np.ascontiguousarray(cm).astype(ml_dtypes.bfloat16),
        })
        maps.append(m)
    return maps, owns


def kernel(**inputs):
    maps, owns = host_inputs(inputs)
    nc = build()
    res = run_bass_kernel_spmd(nc, maps, core_ids=list(range(8)))
    out = np.zeros((2, S, D), np.float32)
    for c in range(8):
        b, own = owns[c]
        out[b, own] = np.asarray(res.results[c]["out"], np.float32)
    return out
```

```python
import math
import numpy as np
import ml_dtypes
import concourse.bass as bass
import concourse.mybir as mybir
from concourse.bass_utils import run_bass_kernel_spmd

F32 = mybir.dt.float32
BF16 = mybir.dt.bfloat16
ALU = mybir.AluOpType
AF = mybir.ActivationFunctionType
AX = mybir.AxisListType

S = 8192
D = 1024
DFF = 4096
NOWN = 2048
EPS = 1e-6
NEG = -30000.0
N_IT = 20
IDX_SCALE = (8 ** -0.5) * (64 ** -0.5)
LAM_INIT = 0.8 - 0.6 * math.exp(-0.3 * 0)

KA, KD, KI, VA, VD = 0, 512, 1024, 1152, 1664
WK_COLS = 2176
QA, QD, QI, WI = 0, 512, 1024, 1536
WQ_COLS = 1544


class Tok:
    __slots__ = ("w", "r", "psum")

    def __init__(self, psum=False):
        self.w = None
        self.r = []
        self.psum = psum


class Eng:
    def __init__(self, name, h, sem):
        self.name = name
        self.h = h
        self.sem = sem
        self.cnt = 0
        self.seen = {}


class KB:
    def __init__(self, nc, sems, ndma=24):
        self.nc = nc
        self.e = {
            "pe": Eng("pe", nc.tensor, sems[0]),
            "act": Eng("act", nc.scalar, sems[1]),
            "dve": Eng("dve", nc.vector, sems[2]),
            "pool": Eng("pool", nc.gpsimd, sems[3]),
            "sp": Eng("sp", nc.sync, None),
        }
        self.dsems = [[s, 0] for s in sems[4:4 + ndma]]
        self.drr = 0

    def _wait(self, eng, ev):
        sem, val = ev
        k = id(sem)
        if eng.seen.get(k, 0) >= val:
            return
        eng.h.wait_ge(sem, val)
        eng.seen[k] = val

    def _deps(self, eng, reads, writes):
        for t in reads:
            if t.w is not None:
                if not (eng.name == "pe" and t.w[0] is eng.sem):
                    self._wait(eng, t.w)
            if t.psum:
                for ev in t.r:
                    if ev[0] is not eng.sem:
                        self._wait(eng, ev)
        for t in writes:
            if t.w is not None:
                if not (eng.name == "pe" and t.w[0] is eng.sem):
                    self._wait(eng, t.w)
            for ev in t.r:
                if ev[0] is eng.sem and eng.name == "pe":
                    continue
                self._wait(eng, ev)

    def op(self, en, fn, reads=(), writes=()):
        eng = self.e[en]
        self._deps(eng, reads, writes)
        inst = fn(eng.h)
        eng.cnt += 1
        inst.then_inc(eng.sem, 1)
        ev = (eng.sem, eng.cnt)
        for t in reads:
            t.r.append(ev)
        for t in writes:
            t.w = ev
            t.r = []
        return inst

    def dma(self, out, in_, reads=(), writes=(), q="sp"):
        eng = self.e[q]
        ds = self.dsems[self.drr]
        self.drr = (self.drr + 1) % len(self.dsems)
        if ds[1] > 0:
            self._wait(eng, (ds[0], ds[1]))
        self._deps(eng, reads, writes)
        inst = eng.h.dma_start(out=out, in_=in_)
        ds[1] += 16
        inst.then_inc(ds[0], 16)
        ev = (ds[0], ds[1])
        for t in reads:
            t.r.append(ev)
        for t in writes:
            t.w = ev
            t.r = []
        return ev

    def barrier(self):
        names = ["pe", "act", "dve", "pool", "sp"]
        for a in names:
            ea = self.e[a]
            for b in names:
                eb = self.e[b]
                if a == b or eb.sem is None or eb.cnt == 0:
                    continue
                self._wait(ea, (eb.sem, eb.cnt))
            for ds in self.dsems:
                if ds[1] > 0:
                    self._wait(ea, (ds[0], ds[1]))


def toks(n):
    return [Tok() for _ in range(n)]


def build(debug=0):
    nc = bass.Bass("TRN2", target_bir_lowering=False)

    def din(name, shape, dt=F32):
        return nc.dram_tensor(name, list(shape), dt, kind="ExternalInput").ap()

    xT_d = din("xT", [D, S])
    xTo_d = din("xTo", [D, NOWN])
    xo_d = din("xo", [NOWN, D])
    wk_d = din("wk", [D, WK_COLS])
    wq_d = din("wq", [D, WQ_COLS])
    wout_d = din("wout", [D, D])
    wup_d = din("wup", [D, DFF])
    wdn_d = din("wdn", [DFF, D])
    gpm_d = din("gpm", [128, 8])
    gpl_d = din("gpl", [128, 8])
    gpost_d = din("gpost", [2, D])
    gds_d = din("gds", [128, 1])
    lam_d = din("lam", [4, 64])
    cos_d = din("cosf", [128, S])
    sin_d = din("sinf", [128, S])
    coso_d = din("coso", [128, NOWN])
    sino_d = din("sino", [128, NOWN])
    cm_d = din("cmask", [128, 4, 2048], BF16)
    id_d = din("ident", [128, 128], BF16)
    pm_d = din("permm", [128, 128], BF16)
    p2_d = din("pow2", [128, N_IT + 1])

    out_d = nc.dram_tensor("out", [NOWN, D], F32, kind="ExternalOutput").ap()
    kaT_s = nc.dram_tensor("kaT_s", [4, 128, S], BF16, kind="Internal").ap()
    kdT_s = nc.dram_tensor("kdT_s", [4, 128, S], BF16, kind="Internal").ap()
    va_s = nc.dram_tensor("va_s", [64, 128, 512], BF16, kind="Internal").ap()
    vd_s = nc.dram_tensor("vd_s", [64, 128, 512], BF16, kind="Internal").ap()
    q_s = nc.dram_tensor("q_s", [3, 4, 128, NOWN], BF16, kind="Internal").ap()
    mix_s = nc.dram_tensor("mix_s", [8, 128, NOWN], BF16, kind="Internal").ap()
    dbg = {}
    if debug:
        dbg["kiT"] = nc.dram_tensor("dbg_kiT", [128, S], BF16, kind="ExternalOutput").ap()
        dbg["kaT"] = nc.dram_tensor("dbg_kaT", [4, 128, S], BF16, kind="ExternalOutput").ap()
        dbg["va"] = nc.dram_tensor("dbg_va", [64, 128, 512], BF16, kind="ExternalOutput").ap()
        dbg["qaT"] = nc.dram_tensor("dbg_qaT", [128, 4, NOWN], BF16, kind="ExternalOutput").ap()
        dbg["wtok"] = nc.dram_tensor("dbg_wtok", [128, 16, 8], F32, kind="ExternalOutput").ap()
        dbg["mixT"] = nc.dram_tensor("dbg_mixT", [128, 8, NOWN], BF16, kind="ExternalOutput").ap()
        dbg["thr"] = nc.dram_tensor("dbg_thr", [128, 16], F32, kind="ExternalOutput").ap()

    from contextlib import ExitStack
    es = ExitStack()
    with es:
        sems = [es.enter_context(nc.semaphore(f"s{i}")) for i in range(4 + 24)]
        kb = KB(nc, sems)
        op, dma = kb.op, kb.dma

        def sb(name, shape, dt, stack=es):
            return stack.enter_context(nc.sbuf_tensor(name, list(shape), dt))

        ps = [es.enter_context(nc.psum_tensor(f"ps{i}", [128, 512], F32)) for i in range(8)]
        pst = [Tok(psum=True) for _ in range(8)]

        ident = sb("ident_sb", [128, 128], BF16); t_ident = Tok()
        ones_bf = sb("ones_bf", [128, 128], BF16); t_ones = Tok()
        neglam = sb("neglam", [128, 1], F32); t_lam = Tok()
        gds = sb("gds_sb", [128, 1], F32); t_gds = Tok()
        pow2 = sb("pow2_sb", [128, N_IT + 1], F32); t_p2 = Tok()
        epst = sb("epst", [128, 1], F32); t_eps = Tok()
        t_mix = [[Tok() for _ in range(4)] for _ in range(8)]

        dma(ident[:], id_d[:, :], writes=[t_ident])
        permt = sb("permt", [128, 128], BF16); t_perm = Tok()
        dma(permt[:], pm_d[:, :], writes=[t_perm])
        dma(pow2[:], p2_d[:, :], writes=[t_p2])
        op("pool", lambda e: e.memset(ones_bf[:], 1.0), writes=[t_ones])
        op("pool", lambda e: e.memset(epst[:], EPS), writes=[t_eps])

        with ExitStack() as s0:
            lamt = sb("lamt", [128, 4, 64], F32, s0); t_lamt = Tok()
            lprod = sb("lprod", [128, 2, 64], F32, s0); t_lp = Tok()
            lsum = sb("lsum", [128, 2], F32, s0); t_ls = Tok()
            lexp = sb("lexp", [128, 2], F32, s0); t_le = Tok()
            for r in range(4):
                dma(lamt[:, r, :], lam_d[r:r + 1, :].partition_broadcast(128), writes=[t_lamt])
            op("dve", lambda e: e.tensor_tensor(out=lprod[:, 0, :], in0=lamt[:, 0, :], in1=lamt[:, 1, :], op=ALU.mult),
               reads=[t_lamt], writes=[t_lp])
            op("dve", lambda e: e.tensor_tensor(out=lprod[:, 1, :], in0=lamt[:, 2, :], in1=lamt[:, 3, :], op=ALU.mult),
               reads=[t_lamt], writes=[t_lp])
            op("dve", lambda e: e.tensor_reduce(out=lsum[:, :], in_=lprod[:, :, :], axis=AX.X, op=ALU.add),
               reads=[t_lp], writes=[t_ls])
            op("act", lambda e: e.activation(out=lexp[:, :], in_=lsum[:, :], func=AF.Exp), reads=[t_ls], writes=[t_le])
            op("dve", lambda e: e.tensor_tensor(out=neglam[:, :], in0=lexp[:, 1:2], in1=lexp[:, 0:1], op=ALU.subtract),
               reads=[t_le], writes=[t_lam])
            op("dve", lambda e: e.tensor_scalar(out=neglam[:, :], in0=neglam[:, :], scalar1=-LAM_INIT, scalar2=None, op0=ALU.add),
               reads=[t_lam], writes=[t_lam])
            dma(gds[:], gds_d[:, :], writes=[t_gds])
            op("dve", lambda e: e.tensor_scalar(out=gds[:, :], in0=gds[:, :], scalar1=1.0 - LAM_INIT, scalar2=None, op0=ALU.mult),
               reads=[t_gds], writes=[t_gds])
            kb.barrier()

        s12 = es.enter_context(ExitStack())
        kiT = sb("kiT", [128, S], BF16, s12); t_ki = toks(16)
        t_qs = [[[Tok() for _ in range(4)] for _ in range(4)] for _ in range(3)]
        wtok = sb("wtok", [128, 16, 8], F32, s12); t_wtok = toks(16)
        t_kas = [[Tok() for _ in range(16)] for _ in range(4)]
        t_kds = [[Tok() for _ in range(16)] for _ in range(4)]
        t_vas = toks(64)
        t_vds = toks(64)

        def phase1():
            with ExitStack() as s1:
                sfx = "_p1"
                gpm = sb("gpm_sb" + sfx, [128, 8], F32, s1); t_gpm = Tok()
                dma(gpm[:], gpm_d[:, :], writes=[t_gpm])
                Wk = sb("Wk", [128, 8, WK_COLS], BF16, s1); twk = toks(8)
                Wq = sb("Wq", [128, 8, WQ_COLS], BF16, s1); twq = toks(8)
                NW1 = 4
                wst = [sb(f"wst{i}" + sfx, [128, 1024], F32, s1) for i in range(NW1)]; t_wst = toks(NW1)
                n = 0
                for (w_d, Wb, twb, ncol) in ((wk_d, Wk, twk, WK_COLS), (wq_d, Wq, twq, WQ_COLS)):
                    w_v = w_d.rearrange("(t p) c -> t p c", p=128)
                    for t in range(8):
                        for c0 in range(0, ncol, 1024):
                            c1 = min(ncol, c0 + 1024)
                            bq = n % NW1
                            dma(wst[bq][:, :c1 - c0], w_v[t, :, c0:c1], writes=[t_wst[bq]])
                            en = ("dve", "pool", "act")[n % 3]
                            if en == "act":
                                op(en, lambda e: e.activation(out=Wb[:, t, c0:c1], in_=wst[bq][:, :c1 - c0], func=AF.Copy,
                                                              scale=gpm[:, t:t + 1]),
                                   reads=[t_wst[bq], t_gpm], writes=[twb[t]])
                            else:
                                op(en, lambda e: e.tensor_scalar(
                                    out=Wb[:, t, c0:c1], in0=wst[bq][:, :c1 - c0], scalar1=gpm[:, t:t + 1], scalar2=None,
                                    op0=ALU.mult), reads=[t_wst[bq], t_gpm], writes=[twb[t]])
                            n += 1

                xc = [sb(f"xc{i}" + sfx, [128, 8, 512], F32, s1) for i in range(2)]; t_xc = toks(2)
                sq = sb("sq" + sfx, [128, 8, 512], BF16, s1); t_sq = Tok()
                rs1 = sb("rs1" + sfx, [128, 512], F32, s1); t_rs1 = Tok()
                rstd = sb("rstd" + sfx, [128, 512], F32, s1); t_rstd = Tok()
                hT = [sb(f"hT{i}" + sfx, [128, 8, 512], BF16, s1) for i in range(2)]; t_hT = [toks(8), toks(8)]
                cst = [sb(f"cst{i}" + sfx, [128, 512], F32, s1) for i in range(2)]; t_cst = toks(2)
                sst = [sb(f"sst{i}" + sfx, [128, 512], F32, s1) for i in range(2)]; t_sst = toks(2)
                r1 = [sb(f"r1_{i}" + sfx, [128, 512], F32, s1) for i in range(2)]; t_r1 = toks(2)
                r2 = [sb(f"r2_{i}" + sfx, [128, 512], F32, s1) for i in range(2)]; t_r2 = toks(2)
                kst = [sb(f"kst{i}" + sfx, [128, 512], BF16, s1) for i in range(4)]; t_kst = toks(4)
                vst = [sb(f"vst{i}" + sfx, [128, 512], BF16, s1) for i in range(4)]; t_vst = toks(4)
                pbf = [sb(f"pbf{i}" + sfx, [128, 512], BF16, s1) for i in range(2)]; t_pbf = toks(2)

                xT_v = xT_d.rearrange("(t p) n -> p t n", p=128)
                xTo_v = xTo_d.rearrange("(t p) n -> p t n", p=128)
                cnt = {"rope": 0, "v": 0, "pair": 0, "k": 0, "ci": 0}
                pend = []

                def norm_chunk(xsrc, csrc, ssrc, c):
                    b = cnt["ci"] % 2
                    cnt["ci"] += 1
                    dma(xc[b][:], xsrc[:, :, c * 512:(c + 1) * 512], writes=[t_xc[b]])
                    dma(cst[b][:], csrc[:, c * 512:(c + 1) * 512], writes=[t_cst[b]])
                    dma(sst[b][:], ssrc[:, c * 512:(c + 1) * 512], writes=[t_sst[b]])
                    op("act", lambda e: e.activation(out=sq[:], in_=xc[b][:], func=AF.Square),
                       reads=[t_xc[b]], writes=[t_sq])
                    for t in range(8):
                        op("pe", lambda e: e.matmul(ps[7][:, :], lhsT=ones_bf[:, :], rhs=sq[:, t, :],
                                                    start=(t == 0), stop=(t == 7)),
                           reads=[t_ones, t_sq], writes=[pst[7]])
                    op("dve", lambda e: e.tensor_scalar(out=rs1[:], in0=ps[7][:, :], scalar1=1.0 / D, scalar2=EPS,
                                                        op0=ALU.mult, op1=ALU.add), reads=[pst[7]], writes=[t_rs1])
                    op("act", lambda e: e.activation(out=rs1[:], in_=rs1[:], func=AF.Ln), reads=[t_rs1], writes=[t_rs1])
                    op("act", lambda e: e.activation(out=rstd[:], in_=rs1[:], func=AF.Exp, scale=-0.5),
                       reads=[t_rs1], writes=[t_rstd])
                    for t in range(8):
                        en = "dve" if t % 2 == 0 else "pool"
                        op(en, lambda e: e.tensor_tensor(out=hT[b][:, t, :], in0=xc[b][:, t, :], in1=rstd[:],
                                                         op=ALU.mult),
                           reads=[t_xc[b], t_rstd], writes=[t_hT[b][t]])
                    return b

                def rope_finish(item):
                    (b, pa, pb, k, dst_ap, wtoks, after) = item
                    op("pe", lambda e: e.matmul(ps[pb][:, :], lhsT=permt[:, :], rhs=pbf[k][:], start=True, stop=True),
                       reads=[t_perm, t_pbf[k]], writes=[pst[pb]])
                    op("dve", lambda e: e.tensor_tensor(out=r1[k][:], in0=ps[pa][:, :], in1=cst[b][:], op=ALU.mult),
                       reads=[pst[pa], t_cst[b]], writes=[t_r1[k]])
                    op("dve", lambda e: e.tensor_tensor(out=r2[k][:], in0=ps[pb][:, :], in1=sst[b][:], op=ALU.mult),
                       reads=[pst[pb], t_sst[b]], writes=[t_r2[k]])
                    op("pool", lambda e: e.tensor_tensor(out=dst_ap, in0=r1[k][:], in1=r2[k][:], op=ALU.add),
                       reads=[t_r1[k], t_r2[k]], writes=wtoks)
                    if after is not None:
                        after()

                def rope_tile(b, W, tw, c0, dst_ap, wtoks, after=None):
                    pp = cnt["pair"] % 2
                    cnt["pair"] += 1
                    pa, pb = 2 * pp, 2 * pp + 1
                    for t in range(8):
                        op("pe", lambda e: e.matmul(
                            ps[pa][:, :], lhsT=W[:, t, c0:c0 + 128], rhs=hT[b][:, t, :],
                            start=(t == 0), stop=(t == 7)),
                           reads=[tw[t], t_hT[b][t]], writes=[pst[pa]])
                    k = cnt["rope"] % 2
                    cnt["rope"] += 1
                    op("act", lambda e: e.copy(out=pbf[k][:], in_=ps[pa][:, :]), reads=[pst[pa]], writes=[t_pbf[k]])
                    if pend:
                        rope_finish(pend.pop(0))
                    pend.append((b, pa, pb, k, dst_ap, wtoks, after))

                def rope_flush():
                    while pend:
                        rope_finish(pend.pop(0))

                chunks = [("K", c) for c in range(16)] + [("Q", c) for c in range(4)]

                def do_norm(kind, c):
                    if kind == "K":
                        return norm_chunk(xT_v, cos_d, sin_d, c)
                    return norm_chunk(xTo_v, coso_d, sino_d, c)

                b = do_norm(*chunks[0])
                for idx, (kind, c) in enumerate(chunks):
                    if kind == "K":
                        for (base, spill, tsp) in ((KA, kaT_s, t_kas), (KD, kdT_s, t_kds)):
                            for i in range(4):
                                k = cnt["k"] % 4
                                cnt["k"] += 1

                                def after(k=k, spill=spill, i=i, c=c, tsp=tsp):
                                    dma(spill[i, :, c * 512:(c + 1) * 512], kst[k][:], reads=[t_kst[k]],
                                        writes=[tsp[i][c]])
                                rope_tile(b, Wk, twk, base + 128 * i, kst[k][:], [t_kst[k]], after)
                        rope_tile(b, Wk, twk, KI, kiT[:, c * 512:(c + 1) * 512], [t_ki[c]])
                        rope_flush()
                    else:
                        for (wh, base) in ((0, QA), (1, QD), (2, QI)):
                            for i in range(4):
                                k = cnt["k"] % 4
                                cnt["k"] += 1

                                def after(k=k, wh=wh, i=i, c=c):
                                    dma(q_s[wh, i, :, c * 512:(c + 1) * 512], kst[k][:], reads=[t_kst[k]],
                                        writes=[t_qs[wh][i][c]])
                                rope_tile(b, Wq, twq, base + 128 * i, kst[k][:], [t_kst[k]], after)
                        rope_flush()
                    b_next = do_norm(*chunks[idx + 1]) if idx + 1 < len(chunks) else None
                    if kind == "K":
                        for st in range(4):
                            for (base, spill, tsp) in ((VA, va_s, t_vas), (VD, vd_s, t_vds)):
                                bank = 4 + cnt["v"] % 2
                                k = cnt["v"] % 4
                                cnt["v"] += 1
                                for t in range(8):
                                    op("pe", lambda e: e.matmul(
                                        ps[bank][:, :], lhsT=hT[b][:, t, st * 128:(st + 1) * 128],
                                        rhs=Wk[:, t, base:base + 512], start=(t == 0), stop=(t == 7)),
                                       reads=[twk[t], t_hT[b][t]], writes=[pst[bank]])
                                op("act", lambda e: e.copy(out=vst[k][:], in_=ps[bank][:, :]),
                                   reads=[pst[bank]], writes=[t_vst[k]])
                                dma(spill[4 * c + st, :, :], vst[k][:], reads=[t_vst[k]], writes=[tsp[4 * c + st]])
                    else:
                        for st in range(4):
                            g = 4 * c + st
                            bank = 4 + cnt["v"] % 2
                            cnt["v"] += 1
                            for t in range(8):
                                op("pe", lambda e: e.matmul(
                                    ps[bank][:, 0:8], lhsT=hT[b][:, t, st * 128:(st + 1) * 128],
                                    rhs=Wq[:, t, WI:WI + 8], start=(t == 0), stop=(t == 7)),
                                   reads=[twq[t], t_hT[b][t]], writes=[pst[bank]])
                            op("dve", lambda e: e.tensor_scalar(
                                out=wtok[:, g, :], in0=ps[bank][:, 0:8], scalar1=IDX_SCALE, scalar2=None,
                                op0=ALU.mult), reads=[pst[bank]], writes=[t_wtok[g]])
                    b = b_next
                kb.barrier()

        phase1()

        if debug == 1:
            dma(dbg["kiT"][:, :], kiT[:], reads=t_ki)
            dma(dbg["wtok"][:, :, :], wtok[:], reads=t_wtok)
            for i in range(4):
                dma(dbg["qaT"][:, i, :], q_s[0, i, :, :], reads=t_qs[0][i])
                dma(dbg["kaT"][i, :, :], kaT_s[i, :, :], reads=t_kas[i])
            dma(dbg["va"][:, :, :], va_s[:, :, :], reads=t_vas)
            kb.barrier()
            return nc

        def phase2(slots):
            F8 = mybir.dt.float8e5
            with ExitStack() as s2:
                cmask = sb("cmask8", [128, 4, 2048], F8, s2); t_cm = Tok()
                id8 = sb("id8", [128, 128], F8, s2); t_id8 = Tok()
                with ExitStack() as s2t:
                    cmt = sb("cmask_tmp", [128, 4, 2048], BF16, s2t); t_cmt = Tok()
                    dma(cmt[:], cm_d[:, :, :], writes=[t_cmt])
                    op("dve", lambda e: e.tensor_copy(out=cmask[:], in_=cmt[:], saturate=False),
                       reads=[t_cmt], writes=[t_cm])
                    op("dve", lambda e: e.tensor_copy(out=id8[:], in_=ident[:], saturate=False),
                       reads=[t_ident], writes=[t_id8])
                    kb.barrier()
                QbI = sb("QbI", [128, 8, 512], BF16, s2); t_QbI = Tok()
                QbA = sb("QbA", [128, 8, 512], BF16, s2); t_QbA = Tok()
                QbD = QbA; t_QbD = t_QbA
                for (qb_, tq_) in ((QbI, t_QbI), (QbA, t_QbA)):
                    op("pool", lambda e: e.memset(qb_[:], 0.0), writes=[tq_])

                def load_qpad(qb_, tq_, wh, blk):
                    for t in range(4):
                        for half in range(2):
                            dma(qb_[64 * half:64 * half + 64, 2 * t + half, :],
                                q_s[wh, t, 64 * half:64 * half + 64, blk], reads=t_qs[wh][t], writes=[tq_])
                scores = [sb(f"score{k}", [128, S], F32, s2) for k in range(2)]; t_scs = [toks(16), toks(16)]
                mbr = sb("mbr", [128, 6, S], F8, s2)
                t_mbr = [toks(16) for _ in range(6)]
                msA = [sb(f"msA{k}", [128, 512], BF16, s2) for k in range(2)]; t_msA = toks(2)
                msD = [sb(f"msD{k}", [128, 512], BF16, s2) for k in range(2)]; t_msD = toks(2)
                Dm = [sb(f"Dm{i}", [128, 8, 128], BF16, s2) for i in range(2)]; t_Dm = toks(2)
                Rb = [sb(f"Rb{i}", [128, 512], BF16, s2) for i in range(4)]; t_Rb = toks(4)
                am = sb("am", [128, 2], F32, s2); t_am = Tok()
                steps = sb("steps", [128, N_IT + 1], F32, s2); t_steps = Tok()
                thrv = sb("thrv", [128, 2], F32, s2); t_thr = toks(2)
                thrf = sb("thrf", [128, 16], F32, s2); t_thrf = toks(16)
                cntv = sb("cntv", [128, 1], F32, s2); t_cnt = Tok()
                dv = sb("dv", [128, 1], F32, s2); t_dv = Tok()
                kck = [sb(f"kck{i}", [128, 512], BF16, s2) for i in range(2)]; t_kck = toks(2)
                kcv = [sb(f"kcv{i}", [128, 4, 128], BF16, s2) for i in range(2)]; t_kcv = toks(2)
                kva = [sb(f"kva{i}", [128, 4, 2, 128], BF16, s2) for i in range(2)]; t_kva = toks(2)
                stO = [sb(f"stO{i}", [128, 512], F32, s2) for i in range(2)]; t_stO = toks(2)
                stR = [sb(f"stR{i}", [64, 512], F32, s2) for i in range(2)]; t_stR = toks(2)
                Pb = [sb(f"Pb{i}", [128, 512], BF16, s2) for i in range(4)]; t_Pb = toks(4)
                kdk = [sb(f"kdk{i}", [128, 512], BF16, s2) for i in range(2)]; t_kdk = toks(2)
                kdv = [sb(f"kdv{i}", [128, 4, 128], BF16, s2) for i in range(2)]; t_kdv = toks(2)
                _stg = [sb(f"stg_{k}", [128, 512], F32, s2) for k in range(4)]
                stg = [_stg, _stg]
                _tstg = toks(4)
                t_stg = [_tstg, _tstg]
                _sqb = sb("sqb", [128, 512], BF16, s2); _tsqb = Tok()
                sqb = [_sqb, _sqb]; t_sqb = [_tsqb, _tsqb]
                for i in range(2):
                    op("pool", lambda e: e.memset(kva[i][:], 1.0), writes=[t_kva[i]])
                cc = {"ld": 0, "ld2": 0, "s": 0, "dm": 0}

                def build_Dm(g):
                    k = cc["dm"] % 2
                    cc["dm"] += 1
                    op("dve", lambda e: e.tensor_tensor(
                        out=Dm[k][:], in0=ident[:].unsqueeze(1).to_broadcast([128, 8, 128]),
                        in1=wtok[:, g, :].unsqueeze(2).to_broadcast([128, 8, 128]), op=ALU.mult),
                       reads=[t_ident, t_wtok[g]], writes=[t_Dm[k]])
                    return k

                def gen_2a(i):
                    nk = 2048 * (i + 1)
                    nch = 4 * (i + 1)
                    blk = slice(i * 512, (i + 1) * 512)
                    load_qpad(QbI, t_QbI, 2, blk)
                    dk = build_Dm(4 * i)
                    for qt in range(4):
                        g = 4 * i + qt
                        njobs = 8 * nch
                        score = scores[g % 2]
                        t_sc = t_scs[g % 2]
                        reg = g % 6

                        def emitD(j):
                            kc, h = divmod(j, 8)
                            sbank = 6 + kc % 2
                            op("pe", lambda e: e.matmul(ps[sbank][:, :], lhsT=Dm[dk][:, h, :], rhs=Rb[j % 4][:],
                                                        start=(h == 0), stop=(h == 7)),
                               reads=[t_Dm[dk], t_Rb[j % 4]], writes=[pst[sbank]])
                            if h == 7:
                                sl = slice(kc * 512, (kc + 1) * 512)
                                op("act", lambda e: e.copy(out=score[:, sl], in_=ps[sbank][:, :]),
                                   reads=[pst[sbank]], writes=[t_sc[kc]])

                        for j in range(njobs):
                            kc, h = divmod(j, 8)
                            pb_ = 64 * (h % 2)
                            bank = 4 + j % 2
                            op("pe", lambda e: e.matmul(
                                ps[bank][:, :], lhsT=QbI[:, h, qt * 128:(qt + 1) * 128],
                                rhs=kiT[:, kc * 512:(kc + 1) * 512], start=True, stop=True),
                               reads=[t_QbI, t_ki[kc]], writes=[pst[bank]])
                            op("act", lambda e: e.activation(out=Rb[j % 4][:], in_=ps[bank][:, :], func=AF.Relu),
                               reads=[pst[bank]], writes=[t_Rb[j % 4]])
                            if j >= 1:
                                emitD(j - 1)
                            if h == 7 and kc % 2 == 1:
                                yield "s"
                        emitD(njobs - 1)
                        yield "scored"
                        dk_next = build_Dm(g + 1) if qt < 3 else None
                        op("dve", lambda e: e.tensor_reduce(
                            out=am[:, 0:1], in_=score[:, 0:nk], axis=AX.X, op=ALU.max,
                            apply_absolute_value=True), reads=t_sc[:nch], writes=[t_am])
                        for r in range(4):
                            kc = nch - 4 + r
                            op("dve", lambda e: e.tensor_tensor(
                                out=score[:, kc * 512:(kc + 1) * 512], in0=score[:, kc * 512:(kc + 1) * 512],
                                in1=cmask[:, qt, r * 512:(r + 1) * 512], op=ALU.add),
                               reads=[t_cm, t_sc[kc]], writes=[t_sc[kc]])
                        op("dve", lambda e: e.tensor_scalar(out=am[:, 1:2], in0=am[:, 0:1], scalar1=1.001,
                                                            scalar2=1e-20, op0=ALU.mult, op1=ALU.add),
                           reads=[t_am], writes=[t_am])
                        op("dve", lambda e: e.tensor_scalar(out=steps[:], in0=pow2[:], scalar1=am[:, 1:2],
                                                            scalar2=None, op0=ALU.mult),
                           reads=[t_am, t_p2], writes=[t_steps])
                        op("dve", lambda e: e.memset(thrv[:, 0:1], 0.0), writes=[t_thr[0]])
                        for it in range(N_IT):
                            cur, nxt = it % 2, (it + 1) % 2
                            op("dve", lambda e: e.tensor_scalar(
                                out=mbr[:, reg, 0:nk], in0=score[:, 0:nk], scalar1=thrv[:, cur:cur + 1], scalar2=None,
                                op0=ALU.is_ge, op1=ALU.add, accum_out=cntv[:, 0:1], saturate=False),
                               reads=t_sc[:nch] + [t_thr[cur]], writes=t_mbr[reg][:nch] + [t_cnt])
                            op("dve", lambda e: e.tensor_scalar(out=dv[:], in0=cntv[:], scalar1=255.5, scalar2=0.5,
                                                                op0=ALU.is_ge, op1=ALU.subtract),
                               reads=[t_cnt], writes=[t_dv])
                            op("dve", lambda e: e.scalar_tensor_tensor(
                                out=thrv[:, nxt:nxt + 1], in0=dv[:], scalar=steps[:, it:it + 1], in1=thrv[:, cur:cur + 1],
                                op0=ALU.mult, op1=ALU.add), reads=[t_dv, t_steps, t_thr[cur]], writes=[t_thr[nxt]])
                        fin = N_IT % 2
                        op("dve", lambda e: e.scalar_tensor_tensor(
                            out=thrf[:, g:g + 1], in0=steps[:, N_IT:N_IT + 1], scalar=-1.0, in1=thrv[:, fin:fin + 1],
                            op0=ALU.mult, op1=ALU.add), reads=[t_steps, t_thr[fin]], writes=[t_thrf[g]])
                        op("dve", lambda e: e.tensor_scalar(
                            out=mbr[:, reg, 0:nk], in0=score[:, 0:nk], scalar1=thrf[:, g:g + 1], scalar2=NEG,
                            op0=ALU.is_lt, op1=ALU.mult, saturate=False),
                           reads=t_sc[:nch] + [t_thrf[g]], writes=t_mbr[reg][:nch])
                        dk = dk_next
                        yield "bisected"

                def gen_2b(i):
                    nch = 4 * (i + 1)
                    blk = slice(i * 512, (i + 1) * 512)
                    load_qpad(QbA, t_QbA, 0, blk)
                    for p in range(4):
                        njobs = nch * 8
                        bufs = {}

                        def load_dsa(kc):
                            bb = cc["ld"] % 2
                            cc["ld"] += 1
                            dma(kck[bb][:], kaT_s[p, :, kc * 512:(kc + 1) * 512], reads=[t_kas[p][kc]], writes=[t_kck[bb]])
                            dma(kcv[bb][:], va_s[4 * kc:4 * kc + 4, :, 128 * p:128 * p + 128].rearrange("t p c -> p t c"),
                                reads=t_vas[4 * kc:4 * kc + 4], writes=[t_kcv[bb]])
                            op("pool", lambda e: e.tensor_copy(
                                out=kva[bb][:, :, :, 0:64], in_=kcv[bb][:].rearrange("p t (h d) -> p t h d", h=2)),
                               reads=[t_kcv[bb]], writes=[t_kva[bb]])
                            bufs[kc] = bb

                        def emitPV(j):
                            kc, rem = divmod(j, 8)
                            kt, hh = divmod(rem, 2)
                            bb = bufs[kc]
                            pbuf = j % 4
                            op("pe", lambda e: e.matmul(ps[hh][:, :], lhsT=kva[bb][:, kt, hh, :], rhs=Pb[pbuf][:],
                                                        start=(kc == 0 and kt == 0), stop=(kc == nch - 1 and kt == 3)),
                               reads=[t_kva[bb], t_Pb[pbuf]], writes=[pst[hh]])

                        load_dsa(0)
                        for j in range(njobs):
                            kc, rem = divmod(j, 8)
                            kt, hh = divmod(rem, 2)
                            if rem == 2 and kc + 1 < nch:
                                load_dsa(kc + 1)
                            bb = bufs[kc]
                            sbank = 2 + cc["s"] % 2
                            cc["s"] += 1
                            op("pe", lambda e: e.matmul(
                                ps[sbank][:, :], lhsT=kck[bb][:, kt * 128:(kt + 1) * 128],
                                rhs=QbA[:, 2 * p + hh, :], start=True, stop=False),
                               reads=[t_kck[bb], t_QbA], writes=[pst[sbank]])
                            s0 = kc * 512 + kt * 128
                            for qt in range(4):
                                reg = (4 * i + qt) % 6
                                op("pe", lambda e: e.matmul(
                                    ps[sbank][:, qt * 128:(qt + 1) * 128], lhsT=mbr[:, reg, s0:s0 + 128], rhs=id8[:],
                                    start=False, stop=(qt == 3)),
                                   reads=[t_mbr[reg][kc], t_id8], writes=[pst[sbank]])
                            pbuf = j % 4
                            op("act", lambda e: e.activation(out=Pb[pbuf][:], in_=ps[sbank][:, :], func=AF.Exp,
                                                             scale=0.125), reads=[pst[sbank]], writes=[t_Pb[pbuf]])
                            if j >= 2:
                                emitPV(j - 2)
                            yield "b"
                        for j in range(njobs - 2, njobs):
                            emitPV(j)
                        k = p % 2
                        for hh in range(2):
                            op("act", lambda e: e.copy(out=stO[hh][0:64, :], in_=ps[hh][0:64, :]),
                               reads=[pst[hh]], writes=[t_stO[hh]])
                            op("act", lambda e: e.activation(out=stO[hh][64:128, :], in_=ps[hh][64:128, :], func=AF.Ln),
                               reads=[pst[hh]], writes=[t_stO[hh]])
                            op("act", lambda e: e.activation(out=stR[hh][0:64, :], in_=stO[hh][64:128, :],
                                                             func=AF.Exp, scale=-1.0),
                               reads=[t_stO[hh]], writes=[t_stR[hh]])
                            op("pool", lambda e: e.tensor_tensor(
                                out=msA[k][64 * hh:64 * hh + 64, :], in0=stO[hh][0:64, :], in1=stR[hh][0:64, :],
                                op=ALU.mult), reads=[t_stO[hh], t_stR[hh]], writes=[t_msA[k]])
                        dma(mix_s[p, :, blk], msA[k][:], reads=[t_msA[k]], writes=[t_mix[p][i]])
                    yield "bdone"

                def gen_2c(i):
                    nch = 4 * (i + 1)
                    blk = slice(i * 512, (i + 1) * 512)
                    load_qpad(QbD, t_QbD, 1, blk)
                    pending = []

                    def fin_stage3(h):
                        a = h % 2
                        sbank = 2 + cc["s"] % 2
                        cc["s"] += 1
                        op("act", lambda e: e.activation(out=sqb[a][:], in_=stg[a][0][:], func=AF.Square),
                           reads=[t_stg[a][0]], writes=[t_sqb[a]])
                        op("pe", lambda e: e.matmul(ps[sbank][:, :], lhsT=ones_bf[:], rhs=sqb[a][:], start=True, stop=True),
                           reads=[t_ones, t_sqb[a]], writes=[pst[sbank]])
                        op("act", lambda e: e.activation(out=stg[a][1][:], in_=ps[sbank][:, :], func=AF.Ln,
                                                         scale=1.0 / 128, bias=epst[:, 0:1]),
                           reads=[pst[sbank], t_eps], writes=[t_stg[a][1]])
                        op("act", lambda e: e.activation(out=stg[a][1][:], in_=stg[a][1][:], func=AF.Exp, scale=-0.5),
                           reads=[t_stg[a][1]], writes=[t_stg[a][1]])
                        op("pool", lambda e: e.tensor_scalar(out=stg[a][0][:], in0=stg[a][0][:], scalar1=gds[:, 0:1],
                                                             scalar2=None, op0=ALU.mult),
                           reads=[t_stg[a][0], t_gds], writes=[t_stg[a][0]])
                        op("pool", lambda e: e.tensor_tensor(out=msD[a][:], in0=stg[a][0][:], in1=stg[a][1][:],
                                                             op=ALU.mult),
                           reads=[t_stg[a][0], t_stg[a][1]], writes=[t_msD[a]])
                        dma(mix_s[4 + h, :, blk], msD[a][:], reads=[t_msD[a]], writes=[t_mix[4 + h][i]])

                    for h in range(4):
                        a = h % 2
                        for sm in range(2):
                            njobs = nch * 4
                            bufs = {}

                            def load_diff(kc):
                                bb = cc["ld2"] % 2
                                cc["ld2"] += 1
                                dma(kdk[bb][:], kdT_s[h, :, kc * 512:(kc + 1) * 512],
                                    reads=[t_kds[h][kc]], writes=[t_kdk[bb]])
                                dma(kdv[bb][:], vd_s[4 * kc:4 * kc + 4, :, 128 * h:128 * h + 128].rearrange("t p c -> p t c"),
                                    reads=t_vds[4 * kc:4 * kc + 4], writes=[t_kdv[bb]])
                                bufs[kc] = bb

                            def emitPV2(j):
                                kc, kt = divmod(j, 4)
                                bb = bufs[kc]
                                pbuf = j % 4
                                first = (j == 0)
                                last = (j == njobs - 1)
                                op("pe", lambda e: e.matmul(ps[0][:, :], lhsT=kdv[bb][:, kt, :], rhs=Pb[pbuf][:],
                                                            start=first, stop=last),
                                   reads=[t_kdv[bb], t_Pb[pbuf]], writes=[pst[0]])
                                op("pe", lambda e: e.matmul(ps[1][:, :], lhsT=ones_bf[:], rhs=Pb[pbuf][:],
                                                            start=first, stop=last),
                                   reads=[t_ones, t_Pb[pbuf]], writes=[pst[1]])

                            load_diff(0)
                            pb_ = 64 * sm
                            for j in range(njobs):
                                kc, kt = divmod(j, 4)
                                if kt == 2 and kc + 1 < nch:
                                    load_diff(kc + 1)
                                bb = bufs[kc]
                                sbank = 2 + cc["s"] % 2
                                cc["s"] += 1
                                r = kc - (nch - 4)
                                op("pe", lambda e: e.matmul(
                                    ps[sbank][:, :], lhsT=kdk[bb][:, kt * 128:(kt + 1) * 128],
                                    rhs=QbD[:, 2 * h + sm, :], start=True, stop=(r < 0)),
                                   reads=[t_kdk[bb], t_QbD], writes=[pst[sbank]])
                                if r >= 0:
                                    s0 = r * 512 + kt * 128
                                    for qt in range(4):
                                        op("pe", lambda e: e.matmul(
                                            ps[sbank][:, qt * 128:(qt + 1) * 128], lhsT=cmask[:, qt, s0:s0 + 128],
                                            rhs=id8[:], start=False, stop=(qt == 3)),
                                           reads=[t_cm, t_id8], writes=[pst[sbank]])
                                pbuf = j % 4
                                op("act", lambda e: e.activation(out=Pb[pbuf][:], in_=ps[sbank][:, :], func=AF.Exp,
                                                                 scale=0.125), reads=[pst[sbank]], writes=[t_Pb[pbuf]])
                                if j >= 2:
                                    emitPV2(j - 2)
                                if j == 8 and pending:
                                    fin_stage3(pending.pop(0))
                                yield "c"
                            for j in range(njobs - 2, njobs):
                                emitPV2(j)
                            op("act", lambda e: e.copy(out=stg[a][2 * sm][:], in_=ps[0][:, :]),
                               reads=[pst[0]], writes=[t_stg[a][2 * sm]])
                            op("act", lambda e: e.activation(out=stg[a][2 * sm + 1][:], in_=ps[1][:, :], func=AF.Ln),
                               reads=[pst[1]], writes=[t_stg[a][2 * sm + 1]])
                            op("act", lambda e: e.activation(out=stg[a][2 * sm + 1][:], in_=stg[a][2 * sm + 1][:],
                                                             func=AF.Exp, scale=-1.0),
                               reads=[t_stg[a][2 * sm + 1]], writes=[t_stg[a][2 * sm + 1]])
                        op("pool", lambda e: e.tensor_tensor(out=stg[a][0][:], in0=stg[a][0][:], in1=stg[a][1][:], op=ALU.mult),
                           reads=[t_stg[a][0], t_stg[a][1]], writes=[t_stg[a][0]])
                        op("pool", lambda e: e.tensor_tensor(out=stg[a][2][:], in0=stg[a][2][:], in1=stg[a][3][:], op=ALU.mult),
                           reads=[t_stg[a][2], t_stg[a][3]], writes=[t_stg[a][2]])
                        op("pool", lambda e: e.tensor_scalar(out=stg[a][2][:], in0=stg[a][2][:], scalar1=neglam[:, 0:1],
                                                             scalar2=None, op0=ALU.mult),
                           reads=[t_stg[a][2], t_lam], writes=[t_stg[a][2]])
                        op("pool", lambda e: e.tensor_tensor(out=stg[a][0][:], in0=stg[a][0][:], in1=stg[a][2][:], op=ALU.add),
                           reads=[t_stg[a][0], t_stg[a][2]], writes=[t_stg[a][0]])
                        pending.append(h)
                    yield "cfin"
                    while pending:
                        fin_stage3(pending.pop(0))
                    yield "cdone"

                def drain(g):
                    for _ in g:
                        pass

                def advance(g, until):
                    for x in g:
                        if x == until:
                            return True
                    return False

                import itertools
                slots_l = list(slots)
                nreg = len(slots_l)
                for k in range(nreg + 1):
                    parts = []
                    nB = 0
                    if k >= 1:
                        parts.append(gen_2b(slots_l[k - 1]))
                        nB += 32 * 4 * (slots_l[k - 1] + 1)
                    if k < nreg:
                        parts.append(gen_2c(slots_l[k]))
                        nB += 32 * 4 * (slots_l[k] + 1)
                    gb = itertools.chain(*parts)
                    if k < nreg:
                        ga = gen_2a(slots_l[k])
                        per = (nB + 3) // 4
                        alive = True
                        for qt in range(4):
                            advance(ga, "bisected")
                            j = 0
                            while alive and j < per:
                                x = next(gb, None)
                                if x is None:
                                    alive = False
                                    break
                                if x in ("b", "c"):
                                    j += 1
                        drain(ga)
                    drain(gb)
                kb.barrier()
                if debug == 2:
                    dma(dbg["thr"][:, :], thrf[:], reads=t_thrf)
                    kb.barrier()

        phase2([0, 1] if debug == 2 else [0, 1, 2, 3])
        if debug == 2:
            for f in range(8):
                dma(dbg["mixT"][:, f, :], mix_s[f, :, :], reads=t_mix[f])
            kb.barrier()
            return nc

        s12.close()

        with ExitStack() as s3:
            Wout = sb("Wout", [128, 8, D], BF16, s3); t_wo = toks(8)
            Wup = sb("Wup", [128, 8, DFF], BF16, s3); t_wu = toks(8)
            Wdn = sb("Wdn", [128, 32, D], BF16, s3); t_wd = toks(32)
            gpl = sb("gpl_sb", [128, 8], F32, s3); t_gpl = Tok()
            dma(gpl[:], gpl_d[:, :], writes=[t_gpl])
            with ExitStack() as s3w:
                NW3 = 8
                wst = [sb(f"wst3_{i}", [128, 1024], F32, s3w) for i in range(NW3)]; t_wst = toks(NW3)
                n = 0
                wo_v = wout_d.rearrange("(t p) c -> t p c", p=128)
                wu_v = wup_d.rearrange("(t p) c -> t p c", p=128)
                wd_v = wdn_d.rearrange("(t p) c -> t p c", p=128)
                jobs = [(wo_v[t, :, :], Wout[:, t, :], t_wo[t], None) for t in range(8)]
                jobs += [(wu_v[t, :, c0:c0 + 1024], Wup[:, t, c0:c0 + 1024], t_wu[t], t) for t in range(8)
                         for c0 in range(0, DFF, 1024)]
                jobs += [(wd_v[t, :, :], Wdn[:, t, :], t_wd[t], None) for t in range(32)]
                for (src, dst, tk, gt) in jobs:
                    bq = n % NW3
                    dma(wst[bq][:], src, writes=[t_wst[bq]])
                    en = ("dve", "pool", "act")[n % 3]
                    if gt is not None:
                        if en == "act":
                            op(en, lambda e: e.activation(out=dst, in_=wst[bq][:], func=AF.Copy, scale=gpl[:, gt:gt + 1]),
                               reads=[t_wst[bq], t_gpl], writes=[tk])
                        else:
                            op(en, lambda e: e.tensor_scalar(out=dst, in0=wst[bq][:], scalar1=gpl[:, gt:gt + 1],
                                                             scalar2=None, op0=ALU.mult),
                               reads=[t_wst[bq], t_gpl], writes=[tk])
                    else:
                        if en == "act":
                            op(en, lambda e: e.copy(out=dst, in_=wst[bq][:]), reads=[t_wst[bq]], writes=[tk])
                        else:
                            op(en, lambda e: e.tensor_copy(out=dst, in_=wst[bq][:]), reads=[t_wst[bq]], writes=[tk])
                    n += 1
                kb.barrier()
            gpost = sb("gpost_sb", [128, 2, D], F32, s3); t_gp = Tok()
            for r in range(2):
                dma(gpost[:, r, :], gpost_d[r:r + 1, :].partition_broadcast(128), writes=[t_gp])
            x1s = [sb(f"x1_{k}", [128, 2, D], F32, s3) for k in range(2)]; t_x1s = [toks(2), toks(2)]
            mixl = [sb(f"mixl{k}", [128, 8, 128], BF16, s3) for k in range(2)]; t_mixl = toks(2)
            tmpP = sb("tmpP", [128, D], F32, s3); t_tmpP = Tok()
            tmpE = sb("tmpE", [128, D], F32, s3); t_tmpE = Tok()
            h2 = sb("h2", [128, D], BF16, s3); t_h2 = Tok()
            h2Ts = [sb(f"h2T{k}", [128, 8, 256], BF16, s3) for k in range(2)]; t_h2Ts = [toks(2), toks(2)]
            rr = [sb(f"rr{i}", [128, 256], BF16, s3) for i in range(2)]; t_rr = toks(2)
            u2 = [sb(f"u2{i}", [128, 256], BF16, s3) for i in range(2)]; t_u2 = toks(2)
            ssvP = sb("ssvP", [128, 8], F32, s3); t_ssP = Tok()
            ssvE = sb("ssvE", [128, 8], F32, s3); t_ssE = Tok()
            psT = ps[6][:, :].bitcast(BF16)

            def rstd_from(ssv, t_ss, c0, c1, cres, n):
                if c1 is not None:
                    op("dve", lambda e: e.tensor_tensor(out=ssv[:, cres:cres + 1], in0=ssv[:, c0:c0 + 1],
                                                        in1=ssv[:, c1:c1 + 1], op=ALU.add), reads=[t_ss], writes=[t_ss])
                    c0 = cres
                op("dve", lambda e: e.tensor_scalar(out=ssv[:, cres:cres + 1], in0=ssv[:, c0:c0 + 1], scalar1=1.0 / n,
                                                    scalar2=EPS, op0=ALU.mult, op1=ALU.add), reads=[t_ss], writes=[t_ss])
                op("act", lambda e: e.activation(out=ssv[:, cres:cres + 1], in_=ssv[:, cres:cres + 1], func=AF.Ln),
                   reads=[t_ss], writes=[t_ss])
                op("act", lambda e: e.activation(out=ssv[:, cres:cres + 1], in_=ssv[:, cres:cres + 1], func=AF.Exp,
                                                 scale=-0.5), reads=[t_ss], writes=[t_ss])

            def prologue(gi):
                x1 = x1s[gi % 2]; t_x1 = t_x1s[gi % 2]
                h2T = h2Ts[gi % 2]; t_h2T = t_h2Ts[gi % 2]
                for tt in range(2):
                    tok0 = gi * 256 + tt * 128
                    si = tok0 // 512
                    dma(x1[:, tt, :], xo_d[tok0:tok0 + 128, :], writes=[t_x1[tt]])
                    dma(mixl[tt][:], mix_s[:, :, tok0:tok0 + 128].rearrange("f p n -> p f n"),
                        reads=[t_mix[f][si] for f in range(8)], writes=[t_mixl[tt]])
                    yield
                    for half in range(2):
                        for f in range(8):
                            op("pe", lambda e: e.matmul(ps[6 + half][:, :], lhsT=mixl[tt][:, f, :],
                                                        rhs=Wout[:, f, half * 512:(half + 1) * 512],
                                                        start=(f == 0), stop=(f == 7)),
                               reads=[t_mixl[tt], t_wo[f]], writes=[pst[6 + half]])
                    yield
                    for half in range(2):
                        op("act", lambda e: e.activation(out=tmpP[:, half * 512:(half + 1) * 512], in_=ps[6 + half][:, :],
                                                         func=AF.Square, accum_out=ssvP[:, half:half + 1]),
                           reads=[pst[6 + half]], writes=[t_tmpP, t_ssP])
                    rstd_from(ssvP, t_ssP, 0, 1, 2, D)
                    for half in range(2):
                        op("dve", lambda e: e.scalar_tensor_tensor(
                            out=tmpP[:, half * 512:(half + 1) * 512], in0=ps[6 + half][:, :], scalar=ssvP[:, 2:3],
                            in1=gpost[:, 0, half * 512:(half + 1) * 512], op0=ALU.mult, op1=ALU.mult),
                           reads=[pst[6 + half], t_ssP, t_gp], writes=[t_tmpP])
                    op("dve", lambda e: e.tensor_tensor(out=x1[:, tt, :], in0=x1[:, tt, :], in1=tmpP[:], op=ALU.add),
                       reads=[t_tmpP, t_x1[tt]], writes=[t_x1[tt]])
                    yield
                    op("act", lambda e: e.activation(out=tmpP[:], in_=x1[:, tt, :], func=AF.Square,
                                                     accum_out=ssvP[:, 3:4]), reads=[t_x1[tt]], writes=[t_tmpP, t_ssP])
                    rstd_from(ssvP, t_ssP, 3, None, 4, D)
                    op("dve", lambda e: e.tensor_scalar(out=h2[:], in0=x1[:, tt, :], scalar1=ssvP[:, 4:5], scalar2=None,
                                                        op0=ALU.mult), reads=[t_x1[tt], t_ssP], writes=[t_h2])
                    yield
                    for f in range(8):
                        op("pe", lambda e: e.transpose(out=psT[:, f * 128:(f + 1) * 128], in_=h2[:, f * 128:(f + 1) * 128],
                                                       identity=ident[:]), reads=[t_h2, t_ident], writes=[pst[6]])
                    op("dve", lambda e: e.tensor_copy(out=h2T[:, :, tt * 128:(tt + 1) * 128],
                                                      in_=psT.rearrange("p (f n) -> p f n", f=8)),
                       reads=[pst[6]], writes=[t_h2T[tt]])
                    yield

            def drain3(g):
                for _ in g:
                    pass

            drain3(prologue(0))
            for gi in range(8):
                x1 = x1s[gi % 2]; t_x1 = t_x1s[gi % 2]
                h2T = h2Ts[gi % 2]; t_h2T = t_h2Ts[gi % 2]
                nxt = prologue(gi + 1) if gi + 1 < 8 else iter(())

                def emitDown(ff):
                    k = ff % 2
                    for tt in range(2):
                        for half in range(2):
                            op("pe", lambda e: e.matmul(ps[tt * 2 + half][:, :], lhsT=u2[k][:, tt * 128:(tt + 1) * 128],
                                                        rhs=Wdn[:, ff, half * 512:(half + 1) * 512],
                                                        start=(ff == 0), stop=(ff == 31)),
                               reads=[t_u2[k], t_wd[ff]], writes=[pst[tt * 2 + half]])

                for ff in range(32):
                    k = ff % 2
                    ub = 4 + k
                    for f in range(8):
                        op("pe", lambda e: e.matmul(ps[ub][:, 0:256], lhsT=Wup[:, f, ff * 128:(ff + 1) * 128],
                                                    rhs=h2T[:, f, :], start=(f == 0), stop=(f == 7)),
                           reads=[t_wu[f], t_h2T[0], t_h2T[1]], writes=[pst[ub]])
                    op("act", lambda e: e.activation(out=rr[k][:], in_=ps[ub][:, 0:256], func=AF.Relu),
                       reads=[pst[ub]], writes=[t_rr[k]])
                    op("dve", lambda e: e.tensor_tensor(out=u2[k][:], in0=rr[k][:], in1=rr[k][:], op=ALU.mult),
                       reads=[t_rr[k]], writes=[t_u2[k]])
                    if ff >= 1:
                        emitDown(ff - 1)
                    if ff >= 4 and ff % 2 == 0:
                        next(nxt, None)
                emitDown(31)
                drain3(nxt)

                for tt in range(2):
                    tok0 = gi * 256 + tt * 128
                    for half in range(2):
                        op("act", lambda e: e.activation(out=tmpE[:, half * 512:(half + 1) * 512],
                                                         in_=ps[tt * 2 + half][:, :], func=AF.Square,
                                                         accum_out=ssvE[:, 5 + half:6 + half]),
                           reads=[pst[tt * 2 + half]], writes=[t_tmpE, t_ssE])
                    rstd_from(ssvE, t_ssE, 5, 6, 7, D)
                    for half in range(2):
                        op("dve", lambda e: e.scalar_tensor_tensor(
                            out=tmpE[:, half * 512:(half + 1) * 512], in0=ps[tt * 2 + half][:, :], scalar=ssvE[:, 7:8],
                            in1=gpost[:, 1, half * 512:(half + 1) * 512], op0=ALU.mult, op1=ALU.mult),
                           reads=[pst[tt * 2 + half], t_ssE, t_gp], writes=[t_tmpE])
                    op("dve", lambda e: e.tensor_tensor(out=x1[:, tt, :], in0=x1[:, tt, :], in1=tmpE[:], op=ALU.add),
                       reads=[t_tmpE, t_x1[tt]], writes=[t_x1[tt]])
                    dma(out_d[tok0:tok0 + 128, :], x1[:, tt, :], reads=[t_x1[tt]])
            kb.barrier()
        return nc
    return nc


def host_inputs(inputs):
    x = np.asarray(inputs["x"], np.float32)
    w_in = np.asarray(inputs["w_in"], np.float32)[0]
    sp = np.cumsum([512, 512, 512, 512, 64, 8, 512, 512, 512])
    qa, ka, va, qi, ki, wi, qd, kd, vd = np.split(w_in, sp[:-1], axis=1)

    def perm(w):
        n = w.shape[1] // 64
        w4 = w.reshape(w.shape[0], n, 2, 32)
        return w4[:, :, ::-1, :].reshape(w.shape[0], n * 64)

    wk = np.concatenate([ka, kd, ki, ki, va, vd], axis=1)
    wq = np.concatenate([qa, qd, qi, wi], axis=1)
    assert wk.shape[1] == WK_COLS and wq.shape[1] == WQ_COLS
    wk = np.ascontiguousarray(wk)
    wq = np.ascontiguousarray(wq)

    inv = (1.0 / (np.float32(10000.0) ** (np.arange(0, 64, 2, dtype=np.float32) / np.float32(64)))).astype(np.float32)
    ang = (np.arange(S, dtype=np.float32)[:, None] * inv[None, :]).astype(np.float32)
    cos = np.cos(ang).astype(np.float32)
    sin = np.sin(ang).astype(np.float32)
    cosT = np.concatenate([cos, cos, cos, cos], axis=1).T
    sinT = np.concatenate([-sin, sin, -sin, sin], axis=1).T
    cosT = np.ascontiguousarray(cosT, dtype=np.float32)
    sinT = np.ascontiguousarray(sinT, dtype=np.float32)

    def g8(v):
        return np.ascontiguousarray(np.asarray(v, np.float32)[0].reshape(8, 128).T)

    gpost = np.stack([np.asarray(inputs["g_post_mix"], np.float32)[0], np.asarray(inputs["g_post_mlp"], np.float32)[0]])
    lam = np.stack([np.asarray(inputs[k], np.float32)[0] for k in ("lambda_q1", "lambda_k1", "lambda_q2", "lambda_k2")])
    ident = np.eye(128, dtype=np.float32).astype(ml_dtypes.bfloat16)
    partner = np.arange(128) ^ 32
    permm = np.zeros((128, 128), np.float32)
    permm[partner, np.arange(128)] = 1.0
    permm = permm.astype(ml_dtypes.bfloat16)
    pow2 = np.tile((2.0 ** -np.arange(N_IT + 1, dtype=np.float64)).astype(np.float32)[None, :], (128, 1))
    common = {
        "wk": wk, "wq": wq,
        "wout": np.ascontiguousarray(np.asarray(inputs["w_out"], np.float32)[0]),
        "wup": np.ascontiguousarray(np.asarray(inputs["w_up"], np.float32)[0]),
        "wdn": np.ascontiguousarray(np.asarray(inputs["w_down"], np.float32)[0]),
        "gpm": g8(inputs["g_pre_mix"]), "gpl": g8(inputs["g_pre_mlp"]),
        "gpost": np.ascontiguousarray(gpost),
        "gds": np.ascontiguousarray(np.asarray(inputs["g_diff_sub"], np.float32)[0].reshape(128, 1)),
        "lam": np.ascontiguousarray(lam),
        "cosf": cosT, "sinf": sinT, "ident": ident, "pow2": pow2, "permm": permm,
    }
    maps = []
    owns = []
    for c in range(8):
        b, j = c // 4, c % 4
        own = np.concatenate([np.arange(512) + 512 * (4 * i + j) for i in range(4)])
        owns.append((b, own))
        xb = x[b]
        ql = np.arange(512)[:, None]
        sr = np.arange(2048)[None, :]
        cm = np.where(sr <= 512 * j + ql, 0.0, NEG).astype(np.float32)
        cm = cm.reshape(4, 128, 2048).transpose(1, 0, 2)
        m = dict(common)
        m.update({
            "xT": np.ascontiguousarray(xb.T),
            "xTo": np.ascontiguousarray(xb[own].T),
            "xo": np.ascontiguousarray(xb[own]),
            "coso": np.ascontiguousarray(cosT[:, own]),
            "sino": np.ascontiguousarray(sinT[:, own]),
            "cmask": np.ascontiguousarray(cm).astype(ml_dtypes.bfloat16),
        })
        maps.append(m)
    return maps, owns


def kernel(**inputs):
    maps, owns = host_inputs(inputs)
    nc = build()
    res = run_bass_kernel_spmd(nc, maps, core_ids=list(range(8)))
    out = np.zeros((2, S, D), np.float32)
    for c in range(8):
        b, own = owns[c]
        out[b, own] = np.asarray(res.results[c]["out"], np.float32)
    return out
```
